# Optimizing a Trainium2 kernel written in Bass

```python
import jax, jax.numpy as jnp
from jax import lax
import numpy as np

D_MODEL = 2048
BATCH = 2
SEQ = 8192
DEPTH = 1

CHUNK = 64
D_BRANCH = D_MODEL // 2
HEAD_DIM = 64
RWKV_HEADS = D_BRANCH // HEAD_DIM
ATTN_HEADS = D_BRANCH // HEAD_DIM
LORA_DECAY = 64
LORA_ICLR = 64
DECAY_SCALE = 0.606531
PAST_CHUNKS = 8
PAST = PAST_CHUNKS * CHUNK
BAND = (PAST_CHUNKS + 1) * CHUNK
REL_CLIP = 256
N_BRANCH = 2
NORM_EPS = 1e-6
GN_EPS = 64e-5
IN_SIZES = (D_BRANCH,) * 8 + (D_MODEL,) * 2
D_IN = sum(IN_SIZES)

kernel_name = "hybrid_rwkv7_chunkattn_gated_block"


def _rms_norm(x, g):
    xf = x.astype(jnp.float32)
    y = xf * lax.rsqrt(jnp.mean(xf * xf, axis=-1, keepdims=True) + NORM_EPS)
    return (y * g.astype(jnp.float32)).astype(x.dtype)


def _shift(x):
    return jnp.pad(x, ((0, 0), (1, 0), (0, 0)))[:, :-1]


def _rwkv7_mixer(h, p_r, p_k, p_v, mu_rkv, mu_wa, w0, w1, w2, a0, a1, a2,
                 k_k, k_a, r_k, ln_x_g, ln_x_b):
    B, S, _ = h.shape
    H, N = RWKV_HEADS, HEAD_DIM
    f32 = jnp.float32
    r = p_r + (_shift(p_r) - p_r) * mu_rkv[0]
    k = p_k + (_shift(p_k) - p_k) * mu_rkv[1]
    v = p_v + (_shift(p_v) - p_v) * mu_rkv[2]
    dh = _shift(h) - h
    xw = h + dh * mu_wa[0]
    xa = h + dh * mu_wa[1]
    w = jnp.exp(-DECAY_SCALE * jax.nn.sigmoid((w0 + jnp.tanh(xw @ w1) @ w2).astype(f32)))
    a = jax.nn.sigmoid((a0 + (xa @ a1) @ a2).astype(f32))
    r = r.astype(f32)
    k = k.astype(f32)
    v = v.astype(f32)
    kk = (k * k_k.astype(f32)).reshape(B, S, H, N)
    kk = kk / jnp.maximum(jnp.sqrt(jnp.sum(kk * kk, axis=-1, keepdims=True)), 1e-12)
    k = k * (1.0 + (a - 1.0) * k_a.astype(f32))
    r4, k4, v4 = (t.reshape(B, S, H, N) for t in (r, k, v))
    w4, a4 = w.reshape(B, S, H, N), a.reshape(B, S, H, N)

    def step(state, inp):
        rt, wt, kt, vt, kkt, at = inp
        sa = jnp.einsum('bhvk,bhk->bhv', state, -kkt)
        state = (state * wt[:, :, None, :] + sa[..., None] * (kkt * at)[:, :, None, :]
                 + vt[..., :, None] * kt[..., None, :])
        yt = jnp.einsum('bhvk,bhk->bhv', state, rt)
        return state, yt

    seq_major = tuple(jnp.swapaxes(t, 0, 1) for t in (r4, w4, k4, v4, kk, a4))
    state0 = jnp.zeros((B, H, N, N), f32)
    _, y = lax.scan(step, state0, seq_major)
    y = jnp.swapaxes(y, 0, 1)
    mean = jnp.mean(y, axis=-1, keepdims=True)
    var = jnp.mean(jnp.square(y - mean), axis=-1, keepdims=True)
    y = ((y - mean) * lax.rsqrt(var + GN_EPS)).reshape(B, S, D_BRANCH)
    y = y * ln_x_g.astype(f32) + ln_x_b.astype(f32)
    bonus = jnp.sum(r4 * k4 * r_k.astype(f32), axis=-1, keepdims=True) * v4
    y = y + bonus.reshape(B, S, D_BRANCH)
    return y.astype(h.dtype)


def _chunk_band_attention(q, k, v, rel_bias):
    B, S, _ = q.shape
    H, Dh = ATTN_HEADS, HEAD_DIM
    n_chunks = S // CHUNK
    q = q.reshape(B, S, H, Dh) * (Dh ** -0.5)
    kp = jnp.pad(k.reshape(B, S, H, Dh), ((0, 0), (PAST, 0), (0, 0), (0, 0)))
    vp = jnp.pad(v.reshape(B, S, H, Dh), ((0, 0), (PAST, 0), (0, 0), (0, 0)))
    qi = jnp.arange(CHUNK)[:, None]
    kj = jnp.arange(BAND)[None, :]
    rel_idx = jnp.clip(qi - kj + PAST, -REL_CLIP, REL_CLIP) + REL_CLIP
    bias = rel_bias[:, rel_idx].astype(jnp.float32)
    key_offsets = jnp.arange(BAND) - PAST

    def one_chunk(c):
        start = c * CHUNK
        qc = lax.dynamic_slice_in_dim(q, start, CHUNK, axis=1)
        kc = lax.dynamic_slice_in_dim(kp, start, BAND, axis=1)
        vc = lax.dynamic_slice_in_dim(vp, start, BAND, axis=1)
        s = jnp.einsum('bqhd,bkhd->bhqk', qc, kc).astype(jnp.float32) + bias
        valid = (start + key_offsets) >= 0
        s = jnp.where(valid[None, None, None, :], s, jnp.float32(-1e30))
        p = jax.nn.softmax(s, axis=-1).astype(vc.dtype)
        return jnp.einsum('bhqk,bkhd->bqhd', p, vc)

    o = lax.map(one_chunk, jnp.arange(n_chunks))
    return jnp.moveaxis(o, 0, 1).reshape(B, S, H * Dh)


def setup_inputs(seed: int = 0) -> dict:
    key = jax.random.key(seed)
    ks = jax.random.split(key, 24)
    L, D, DB, H, N = DEPTH, D_MODEL, D_BRANCH, RWKV_HEADS, HEAD_DIM
    nrm = lambda k, shape, s: jax.random.normal(k, shape, jnp.float32) * s
    return {
        "x": nrm(ks[0], (BATCH, SEQ, D), 1.0),
        "pre_norm_g": 1.0 + nrm(ks[1], (L, D), 0.02),
        "post_norm_g": 1.0 + nrm(ks[2], (L, D), 0.02),
        "w_in": nrm(ks[3], (L, D, D_IN), D ** -0.5),
        "mu_rkv": jax.random.uniform(ks[4], (L, 3, DB), jnp.float32),
        "mu_wa": jax.random.uniform(ks[5], (L, 2, D), jnp.float32),
        "w0": -1.5 + nrm(ks[6], (L, DB), 1.0),
        "w1": nrm(ks[7], (L, D, LORA_DECAY), D ** -0.5),
        "w2": nrm(ks[8], (L, LORA_DECAY, DB), 0.5 * LORA_DECAY ** -0.5),
        "a0": nrm(ks[9], (L, DB), 0.3),
        "a1": nrm(ks[10], (L, D, LORA_ICLR), D ** -0.5),
        "a2": nrm(ks[11], (L, LORA_ICLR, DB), 0.5 * LORA_ICLR ** -0.5),
        "k_k": 0.85 + nrm(ks[12], (L, DB), 0.02),
        "k_a": 1.0 + nrm(ks[13], (L, DB), 0.02),
        "r_k": nrm(ks[14], (L, H, N), 0.1),
        "ln_x_g": 1.0 + nrm(ks[15], (L, DB), 0.02),
        "ln_x_b": nrm(ks[16], (L, DB), 0.02),
        "rel_bias": nrm(ks[17], (L, ATTN_HEADS, 2 * REL_CLIP + 1), 0.1),
        "w_branch_rwkv": nrm(ks[18], (L, DB, D), DB ** -0.5),
        "w_branch_attn": nrm(ks[19], (L, DB, D), DB ** -0.5),
        "b_merge": nrm(ks[20], (L, N_BRANCH, D), 0.1),
        "w_out": nrm(ks[21], (L, D, D), D ** -0.5),
    }


def reference(x, pre_norm_g, post_norm_g, w_in, mu_rkv, mu_wa, w0, w1, w2, a0, a1, a2,
              k_k, k_a, r_k, ln_x_g, ln_x_b, rel_bias, w_branch_rwkv, w_branch_attn,
              b_merge, w_out):
    split_points = np.cumsum(IN_SIZES)[:-1].tolist()
    for l in range(DEPTH):
        h = _rms_norm(x, pre_norm_g[l])
        proj = h @ w_in[l]
        p_r, p_k, p_v, z_r, q_a, k_a_att, v_a, z_a, m_r, m_a = jnp.split(
            proj, split_points, axis=-1)
        y_r = _rwkv7_mixer(h, p_r, p_k, p_v, mu_rkv[l], mu_wa[l], w0[l], w1[l], w2[l],
                           a0[l], a1[l], a2[l], k_k[l], k_a[l], r_k[l],
                           ln_x_g[l], ln_x_b[l]) * jax.nn.silu(z_r)
        y_a = _chunk_band_attention(q_a, k_a_att, v_a, rel_bias[l]) * jax.nn.silu(z_a)
        u_r = y_r @ w_branch_rwkv[l]
        u_a = y_a @ w_branch_attn[l]
        merged = (jax.nn.sigmoid(m_r + b_merge[l, 0]) * u_r
                  + jax.nn.sigmoid(m_a + b_merge[l, 1]) * u_a)
        o = merged @ w_out[l]
        x = x + _rms_norm(o, post_norm_g[l])
    return x
```

```python
import os
import numpy as np
from contextlib import ExitStack
import concourse.bass as bass
import concourse.mybir as mybir
from concourse.bass_utils import run_bass_kernel_spmd

F32 = mybir.dt.float32
BF16 = mybir.dt.bfloat16
ALU = mybir.AluOpType
AF = mybir.ActivationFunctionType
AX = mybir.AxisListType

D = 2048
S = 8192
NB1 = S // 128
CDEC = 0.606531
NR = 6
NEG = -30000.0
GAP = 3
SELF_SYNC = os.environ.get("K_SELF_SYNC") == "1"


class Buf:
    __slots__ = ("name", "w", "r")

    def __init__(self, name):
        self.name = name
        self.w = None
        self.r = {}


class Ctx:
    def __init__(self, nc, st):
        self.nc = nc
        self.st = st
        self.eng = {}
        for name in ["pe", "act", "dve", "pool", "sp"]:
            sem = st.enter_context(nc.semaphore("s_" + name))
            self.eng[name] = dict(sem=sem, n=0, seen={}, prog=[])
        self.dma = {}
        self.spacer = {}

    def dma_sem(self, name):
        if name not in self.dma:
            self.dma[name] = [self.st.enter_context(self.nc.semaphore("d_" + name)), 0]
        return self.dma[name]

    def op(self, eng, fn, reads=(), writes=(), dma=None):
        E = self.eng[eng]
        deps = {}

        def need(tok):
            if tok is None:
                return
            key, sem, val = tok
            if key == eng and (eng == "pe" or (eng != "pool" and not SELF_SYNC)):
                return
            if key not in deps or deps[key][1] < val:
                deps[key] = (sem, val)

        for b in reads:
            need(b.w)
        for b in writes:
            need(b.w)
            for t in b.r.values():
                need(t)
        if dma is None and eng in self.spacer and not SELF_SYNC:
            last = 0
            for b in list(reads) + list(writes):
                for t in ([b.w] if b.w else []) + list(b.r.values()):
                    if t[0] == eng:
                        last = max(last, t[2])
            if last > 0:
                nsp = GAP - (E["n"] + 1 - last)
                for _ in range(max(0, nsp)):
                    E["n"] += 1
                    E["prog"].append(([], self.spacer[eng], None, E["n"]))
        waits = []
        for key, (sem, val) in deps.items():
            if E["seen"].get(key, 0) < val:
                E["seen"][key] = val
                waits.append((key, sem, val))
        if dma is None:
            E["n"] += 1
            tok = (eng, E["sem"], E["n"])
            E["prog"].append((waits, fn, None, E["n"]))
        else:
            dma = dma + "_" + eng
            d = self.dma_sem(dma)
            d[1] += 16
            tok = ("dma:" + dma, d[0], d[1])
            E["prog"].append((waits, fn, d[0], 0))
        for b in reads:
            b.r[tok[0]] = tok
        for b in writes:
            b.w = tok
            b.r = {}
        return tok

    def wait_tok(self, eng, tok):
        E = self.eng[eng]
        key, sem, val = tok
        if key != eng and E["seen"].get(key, 0) < val:
            E["seen"][key] = val
            E["prog"].append(([(key, sem, val)], None, None, 0))

    def barrier(self):
        toks = [(n, E["sem"], E["n"]) for n, E in self.eng.items() if E["n"] > 0]
        toks += [("dma:" + n, d[0], d[1]) for n, d in self.dma.items() if d[1] > 0]
        for n in self.eng:
            for t in toks:
                self.wait_tok(n, t)

    def emit(self):
        nc = self.nc
        needed = {n: set() for n in self.eng}
        for n, E in self.eng.items():
            for waits, fn, dsem, idx in E["prog"]:
                for (key, sem, val) in waits:
                    if key in needed:
                        needed[key].add(val)
        rank = {n: {v: i + 1 for i, v in enumerate(sorted(vs))} for n, vs in needed.items()}
        self.stats = {n: (E["n"], len(needed[n])) for n, E in self.eng.items()}
        with nc.Block() as block:
            def replay(name):
                def body(e):
                    for waits, fn, dsem, idx in self.eng[name]["prog"]:
                        for (key, sem, val) in waits:
                            e.wait_ge(sem, rank[key][val] if key in rank else val)
                        if fn is not None:
                            ins = fn(e)
                            if dsem is not None:
                                ins.then_inc(dsem, 16)
                            elif idx in needed[name]:
                                ins.then_inc(self.eng[name]["sem"], 1)
                return body
            block.tensor(replay("pe"))
            block.scalar(replay("act"))
            block.vector(replay("dve"))
            block.gpsimd(replay("pool"))
            block.sync(replay("sp"))


def build(mode="fused", nblk=NB1, n_tt=4, stop=99):
    nc = bass.Bass("TRN2", target_bir_lowering=False)
    dr = lambda name, shape, dt=F32, kind="ExternalInput": nc.dram_tensor(name, shape, dt, kind=kind).ap()
    xb = dr("xb", [S, D])
    w1p = dr("w1p", [128, 16, 2176])
    w2a2 = dr("w2a2", [64, 512])
    bias01 = dr("bias01", [1, 512])
    pvec = dr("pvec", [1, 8 * 256])
    pcol = dr("pcol", [128, 48])
    ident_d = dr("ident", [128, 128])
    tri3_d = dr("tri3", [128, 384])
    mask4_d = dr("mask4", [128, 512])
    maskts4_d = dr("maskts4", [128, 512])
    ch_d = dr("ch", [128, 64])
    biasT_d = dr("biasT", [128, 4 * 640])
    maskA_d = dr("maskA", [128, 640])
    if mode == "p1":
        yT_d = dr("yT", [512, S], BF16, kind="ExternalOutput")

    with ExitStack() as st:
        cx = Ctx(nc, st)
        op = cx.op

        def sb(name, shape, dt=F32):
            t = st.enter_context(nc.sbuf_tensor(name, shape, dt))
            return t, Buf(name)

        def ps(name, shape, dt=F32):
            t = st.enter_context(nc.psum_tensor(name, shape, dt))
            return t, Buf(name)

        SCRd, _ = sb("SCRd", [128, 256])
        SCRa, _ = sb("SCRa", [128, 256])
        cx.spacer["dve"] = lambda e: e.memset(SCRd[:], 0.0)
        cx.spacer["act"] = lambda e: e.activation(out=SCRa[:], in_=SCRd[:], func=AF.Copy)
        op("dve", lambda e: e.memset(SCRd[:], 0.0))
        WA, bWA = sb("WA", [128, 16, 2176], BF16)
        WB, bWB = sb("WB", [128, 16, 896], BF16)
        W2, bW2 = sb("W2", [64, 512])
        B01, bB01 = sb("B01", [128, 512])
        ONES1, bONES1 = sb("ONES1", [1, 128])
        PV_, bPV = sb("PV", [128, 5 * 256])
        PC, bPC = sb("PC", [128, 48])
        GS, bGS = sb("GS", [128, 64])
        IDF, bIDF = sb("IDF", [128, 128])
        IDB, bIDB = sb("IDB", [128, 128], BF16)
        TRI, bTRI = sb("TRI", [128, 384])
        MK4, bMK4 = sb("MK4", [128, 512])
        MKTS, bMKTS = sb("MKTS", [128, 512])
        CHt, bCH = sb("CH", [128, 64])
        BM, bBM = sb("BM", [128, 4 * 640], BF16)
        EPS, bEPS = sb("EPS", [128, 2])
        st_outer = st
        st = ExitStack()
        st.__enter__()
        wst = [sb("wst%d" % i, [128, 2176]) for i in range(2)]
        bmst, bbmst = sb("bmst", [128, 4 * 640])
        mast, bmast = sb("mast", [128, 640])
        MU3, bMU3 = sb("MU3", [128, 768])
        OM3, bOM3 = sb("OM3", [128, 768])
        PC8, bPC8 = sb("PC8", [128, 16])

        def ld(eng, dst, bdst, src, name):
            op(eng, lambda e: e.dma_start(out=dst, in_=src), writes=[bdst], dma=name)

        ld("sp", W2[:], bW2, w2a2[:, :], "c0")
        ld("sp", B01[:], bB01, bias01[0:1, :].partition_broadcast(128), "c1")
        ld("sp", PV_[:], bPV, pvec[0:1, 768:2048].partition_broadcast(128), "c2")
        ld("sp", MU3[:], bMU3, pvec[0:1, 0:768].partition_broadcast(128), "c11")
        ld("sp", PC[:], bPC, pcol[:, :], "c3")
        ld("sp", IDF[:], bIDF, ident_d[:, :], "c4")
        ld("sp", TRI[:], bTRI, tri3_d[:, :], "c5")
        ld("sp", MK4[:], bMK4, mask4_d[:, :], "c6")
        ld("sp", MKTS[:], bMKTS, maskts4_d[:, :], "c7")
        ld("sp", CHt[:], bCH, ch_d[:, :], "c8")
        ld("sp", bmst[:], bbmst, biasT_d[:, :], "c9")
        ld("sp", mast[:], bmast, maskA_d[:, :], "c10")

        op("dve", lambda e: e.memset(ONES1[:], 1.0), writes=[bONES1])
        op("dve", lambda e: e.memset(EPS[:, 0:1], 1e-6), writes=[bEPS])
        op("dve", lambda e: e.memset(EPS[:, 1:2], 64e-5), writes=[bEPS])
        op("dve", lambda e: e.tensor_copy(out=IDB[:], in_=IDF[:]), reads=[bIDF], writes=[bIDB])
        op("dve", lambda e: e.tensor_scalar(out=OM3[:], in0=MU3[:], scalar1=-1.0, scalar2=1.0, op0=ALU.mult, op1=ALU.add),
           reads=[bMU3], writes=[bOM3])
        op("dve", lambda e: e.tensor_scalar(out=PC8[:], in0=PC[:, 0:16], scalar1=0.125, scalar2=None, op0=ALU.mult), reads=[bPC], writes=[bPC8])
        for i, (src_c, inv) in enumerate([(16, True), (16, False), (32, True), (32, False)]):
            if inv:
                op("dve", lambda e, i=i, src_c=src_c: e.tensor_scalar(out=GS[:, i * 16:(i + 1) * 16], in0=PC[:, src_c:src_c + 16],
                                                                    scalar1=-1.0, scalar2=1.0, op0=ALU.mult, op1=ALU.add),
                   reads=[bPC], writes=[bGS])
                op("dve", lambda e, i=i: e.tensor_tensor(out=GS[:, i * 16:(i + 1) * 16], in0=GS[:, i * 16:(i + 1) * 16], in1=PC[:, 0:16], op=ALU.mult),
                   reads=[bPC], writes=[bGS])
            else:
                op("dve", lambda e, i=i, src_c=src_c: e.tensor_tensor(out=GS[:, i * 16:(i + 1) * 16], in0=PC[:, src_c:src_c + 16], in1=PC[:, 0:16], op=ALU.mult),
                   reads=[bPC], writes=[bGS])
        op("dve", lambda e: e.tensor_tensor(out=BM[:].rearrange("p (h c) -> p h c", h=4), in0=bmst[:].rearrange("p (h c) -> p h c", h=4),
                                            in1=mast[:].unsqueeze(1).to_broadcast([128, 4, 640]), op=ALU.add),
           reads=[bbmst, bmast], writes=[bBM])
        for kt in range(16):
            wt, bwt = wst[kt % 2]
            ld("sp" if kt % 2 == 0 else "pool", wt[:], bwt, w1p[:, kt, :], "w%d" % (kt % 2))
            g = PC[:, kt:kt + 1]
            op("dve", lambda e, wt=wt, g=g, kt=kt: e.scalar_tensor_tensor(out=WA[:, kt, 0:768], in0=wt[:, 0:768], scalar=g, in1=OM3[:], op0=ALU.mult, op1=ALU.mult),
               reads=[bwt, bOM3, bPC], writes=[bWA])
            op("dve", lambda e, wt=wt, g=g, kt=kt: e.scalar_tensor_tensor(out=WB[:, kt, 0:768], in0=wt[:, 0:768], scalar=g, in1=MU3[:], op0=ALU.mult, op1=ALU.mult),
               reads=[bwt, bMU3, bPC], writes=[bWB])
            for i, (dst, c0) in enumerate([(WA, 768), (WB, 768), (WA, 832), (WB, 832)]):
                op("dve", lambda e, wt=wt, dst=dst, c0=c0, i=i, kt=kt: e.tensor_scalar(out=dst[:, kt, c0:c0 + 64], in0=wt[:, c0:c0 + 64],
                                                                                      scalar1=GS[:, i * 16 + kt:i * 16 + kt + 1], scalar2=None, op0=ALU.mult),
                   reads=[bwt, bGS], writes=[bWA if dst is WA else bWB])
            op("act", lambda e, wt=wt, g=g, kt=kt: e.activation(out=WA[:, kt, 896:1664], in_=wt[:, 896:1664], func=AF.Copy, scale=g),
               reads=[bwt, bPC], writes=[bWA])
            op("dve", lambda e, wt=wt, g=g, kt=kt: e.tensor_scalar(out=WA[:, kt, 1664:1920], in0=wt[:, 1664:1920], scalar1=PC8[:, kt:kt + 1], scalar2=None, op0=ALU.mult),
               reads=[bwt, bPC8], writes=[bWA])
            op("act", lambda e, wt=wt, g=g, kt=kt: e.activation(out=WA[:, kt, 1920:2176], in_=wt[:, 1920:2176], func=AF.Copy, scale=g),
               reads=[bwt, bPC], writes=[bWA])

        cx.barrier()
        st.__exit__(None, None, None)
        st = st_outer
        xt = [sb("xt%d" % i, [128, D]) for i in range(1)]
        xs, bxs = sb("xs", [128, D], BF16)
        st8, bst8 = sb("st8", [128, 8])
        hT = [sb("hT%d" % i, [128, 16, 128], BF16) for i in range(2)]
        hBt, bhB = sb("hB", [128, 16, 128], BF16)
        rk_t, brk = sb("rk_t", [128, 512])
        v_t, bv = sb("v_t", [128, 256])
        vbf, bvbf = sb("vbf", [128, 256], BF16)
        l1T, bl1T = sb("l1T", [64, 256])
        sgin, bsgin = sb("sgin", [128, 512])
        sga, bsga = sb("sga", [128, 512])
        Eg, bEg = sb("Eg", [128, 768])
        gi, bgi = sb("gi", [128, 256])
        gLT, bgLT = sb("gLT", [64, 8])
        szr, bszr = sb("szr", [128, 256])
        sza, bsza = sb("sza", [128, 256])
        qkb, bqkb = sb("qkb", [128, 512], BF16)
        vring, bvring = sb("vring", [128, NR, 256], BF16)
        ONEC, bONEC = sb("ONEC", [128, 2], BF16)
        bvr = [Buf("vr%d" % i) for i in range(NR)]
        kTr, bkTr = sb("kTr", [128, 2, NR, 128], BF16)
        bkr = [Buf("kr%d" % i) for i in range(NR)]
        qT, bqT = sb("qT", [128, 2, 128], BF16)
        tmpA, btmpA = sb("tmpA", [128, 256])
        tmpB, btmpB = sb("tmpB", [128, 256])
        kk_t, bkk = sb("kk_t", [128, 256])
        k_t, bk = sb("k_t", [128, 256])
        ka_t, bka = sb("ka_t", [128, 256])
        s4, bs4 = sb("s4", [128, 16])
        TA, bTA = sb("TA", [128, 1024], BF16)
        btl, bbtl = sb("btl", [128, 256], BF16)
        ktl, bktl = sb("ktl", [128, 256], BF16)
        FT, bFT = sb("FT", [128, 1024], BF16)
        AT = [sb("AT%d" % h, [128, 512], BF16) for h in range(4)]
        XS = [sb("XS%d" % i, [128, 512], BF16) for i in range(2)]
        YS = [sb("YS%d" % i, [128, 512], BF16) for i in range(2)]
        Z = [sb("Z%d" % i, [128, 4, 128], BF16) for i in range(2)]
        DG, bDG = sb("DG", [64, 4, 128])
        PQs, bPQs = sb("PQs", [64, 2, 4, 128])
        RP = [sb("RP%d" % i, [64, 4, 128], BF16) for i in range(2)]
        TS = [sb("TS%d" % i, [64, 4, 64]) for i in range(2)]
        TSb = [sb("TSb%d" % i, [64, 4, 64], BF16) for i in range(2)]
        Yt, bYt = sb("Yt", [128, 256])
        Ysq, bYsq = sb("Ysq", [128, 256])
        yrb, byrb = sb("yrb", [128, 256], BF16)
        PT = [sb("PT%d" % h, [128, 5, 128], BF16) for h in range(2)]
        ya_t, bya = sb("ya_t", [128, 256])
        yab, byab = sb("yab", [128, 256], BF16)
        yTs = [sb("yTs%d" % i, [128, 4, 128], BF16) for i in range(2)]

        PB = [ps("PB%d" % i, [128, 512]) for i in range(3)]
        TBt, bTB = ps("TB", [128, 1024], BF16)
        G = [ps("G%d" % i, [128, 512]) for i in range(4)]

        op("dve", lambda e: e.memset(TS[0][0][:], 0.0), writes=[TS[0][1]])
        op("dve", lambda e: e.memset(TSb[0][0][:], 0.0), writes=[TSb[0][1]])
        op("dve", lambda e: e.memset(ONEC[:], 1.0), writes=[bONEC])
        op("dve", lambda e: e.memset(DG[:], 0.0), writes=[bDG])
        for i in range(2):
            op("dve", lambda e, i=i: e.memset(RP[i][0][:], 0.0), writes=[RP[i][1]])

        PVc = lambda i: PV_[:, i * 256:(i + 1) * 256]
        KKp, KAp, RKp, LNG, LNB = PVc(0), PVc(1), PVc(2), PVc(3), PVc(4)
        h4 = lambda ap: ap.rearrange("p (h n) -> p h n", h=4)
        bc4 = lambda ap: ap.unsqueeze(2).to_broadcast([128, 4, 64])
        gcnt = [0]

        def gbank():
            gcnt[0] += 1
            return G[gcnt[0] % 4]

        def do_block(blk):
            xtile, bx = xt[0]
            hcur, bh = hT[blk % 2]
            hprev, bhp = hT[(blk + 1) % 2]
            ld("sp", xtile[:], bx, xb[blk * 128:(blk + 1) * 128, :], "x%d" % (blk % 2))
            op("dve", lambda e: e.memset(st8[:, 0:1], 0.0), writes=[bst8])
            op("act", lambda e: e.activation(out=xs[:], in_=xtile[:], func=AF.Square, accum_out=st8[:, 0:1]), reads=[bx], writes=[bxs, bst8])
            op("act", lambda e: e.activation(out=st8[:, 1:2], in_=st8[:, 0:1], func=AF.Sqrt, bias=EPS[:, 0:1], scale=1.0 / D), reads=[bEPS], writes=[bst8])
            op("dve", lambda e: e.reciprocal(out=st8[:, 2:3], in_=st8[:, 1:2]), writes=[bst8])
            op("dve", lambda e: e.tensor_scalar(out=xs[:], in0=xtile[:], scalar1=st8[:, 2:3], scalar2=None, op0=ALU.mult), reads=[bx, bst8], writes=[bxs])
            for half in range(2):
                for j in range(8):
                    kt = half * 8 + j
                    op("pe", lambda e, j=j, kt=kt: e.transpose(TBt[:, j * 128:(j + 1) * 128], xs[:, kt * 128:(kt + 1) * 128], IDB[:]),
                       reads=[bxs, bIDB], writes=[bTB])
                op("act" if half == 0 else "dve",
                   (lambda e, half=half: e.activation(out=hcur[:, half * 8:(half + 1) * 8, :], in_=TBt[:].rearrange("p (k t) -> p k t", k=8), func=AF.Copy)) if half == 0 else
                   (lambda e, half=half: e.tensor_copy(out=hcur[:, half * 8:(half + 1) * 8, :], in_=TBt[:].rearrange("p (k t) -> p k t", k=8))),
                   reads=[bTB], writes=[bh])
            if blk == 0:
                op("pool", lambda e: e.memset(hBt[:, :, 0:1], 0.0), writes=[bhB])
            else:
                op("pool", lambda e: e.tensor_copy(out=hBt[:, :, 0:1], in_=hprev[:, :, 127:128]), reads=[bhp], writes=[bhB])
            op("pool", lambda e: e.tensor_copy(out=hBt[:, :, 1:128], in_=hcur[:, :, 0:127]), reads=[bh], writes=[bhB])
            if stop <= 1:
                return
            (P0, bP0), (P1, bP1), (P2, bP2) = PB
            for kt in range(16):
                first = kt == 0
                last = kt == 15
                op("pe", lambda e, kt=kt, first=first: e.matmul(P0[:, :], lhsT=hcur[:, kt, :], rhs=WA[:, kt, 0:512], start=first, stop=False), reads=[bh, bWA], writes=[bP0])
                op("pe", lambda e, kt=kt, first=first: e.matmul(P1[:, 0:256], lhsT=hcur[:, kt, :], rhs=WA[:, kt, 512:768], start=first, stop=False), reads=[bh, bWA], writes=[bP1])
                op("pe", lambda e, kt=kt, last=last: e.matmul(P0[:, :], lhsT=hBt[:, kt, :], rhs=WB[:, kt, 0:512], start=False, stop=last), reads=[bhB, bWB], writes=[bP0])
                op("pe", lambda e, kt=kt, last=last: e.matmul(P1[:, 0:256], lhsT=hBt[:, kt, :], rhs=WB[:, kt, 512:768], start=False, stop=last), reads=[bhB, bWB], writes=[bP1])
            for kt in range(16):
                op("pe", lambda e, kt=kt: e.matmul(P2[:, :], lhsT=hcur[:, kt, :], rhs=WA[:, kt, 896:1408], start=(kt == 0), stop=(kt == 15)), reads=[bh, bWA], writes=[bP2])
            if stop <= 1.2:
                return
            op("dve", lambda e: e.tensor_copy(out=rk_t[:], in_=P0[:, :]), reads=[bP0], writes=[brk])
            op("act", lambda e: e.activation(out=v_t[:], in_=P1[:, 0:256], func=AF.Copy), reads=[bP1], writes=[bv])
            op("dve", lambda e: e.tensor_copy(out=vbf[:], in_=v_t[:]), reads=[bv], writes=[bvbf])
            if stop <= 1.4:
                return
            for kt in range(16):
                op("pe", lambda e, kt=kt: e.matmul(P0[:, :], lhsT=hcur[:, kt, :], rhs=WA[:, kt, 1408:1920], start=(kt == 0), stop=(kt == 15)), reads=[bh, bWA], writes=[bP0])
                op("pe", lambda e, kt=kt: e.matmul(P1[:, 0:256], lhsT=hcur[:, kt, :], rhs=WA[:, kt, 1920:2176], start=(kt == 0), stop=(kt == 15)), reads=[bh, bWA], writes=[bP1])
            if stop <= 1.6:
                return
            slot = blk % NR
            op("act", lambda e: e.activation(out=szr[:], in_=P2[:, 0:256], func=AF.Silu), reads=[bP2], writes=[bszr])
            if stop <= 1.7:
                return
            op("act", lambda e, slot=slot: e.activation(out=vring[:, slot, :], in_=P2[:, 256:512], func=AF.Copy), reads=[bP2], writes=[bvr[slot]])
            if stop <= 1.8:
                return
            op("act", lambda e: e.activation(out=sza[:], in_=P0[:, 0:256], func=AF.Silu), reads=[bP0], writes=[bsza])
            op("act", lambda e: e.activation(out=qkb[:, 0:256], in_=P0[:, 256:512], func=AF.Copy), reads=[bP0], writes=[bqkb])
            op("act", lambda e: e.activation(out=qkb[:, 256:512], in_=P1[:, 0:256], func=AF.Copy), reads=[bP1], writes=[bqkb])

            if stop <= 2:
                return
            (Ga, bGa) = gbank()
            for li in range(2):
                c0 = 768 + li * 64
                for kt in range(16):
                    op("pe", lambda e, Ga=Ga, li=li, c0=c0, kt=kt: e.matmul(Ga[0:64, li * 128:(li + 1) * 128], lhsT=WA[:, kt, c0:c0 + 64], rhs=hcur[:, kt, :], start=(kt == 0), stop=False),
                       reads=[bWA, bh], writes=[bGa])
                    op("pe", lambda e, Ga=Ga, li=li, c0=c0, kt=kt: e.matmul(Ga[0:64, li * 128:(li + 1) * 128], lhsT=WB[:, kt, c0:c0 + 64], rhs=hBt[:, kt, :], start=False, stop=(kt == 15)),
                       reads=[bWB, bhB], writes=[bGa])
            op("act", lambda e, Ga=Ga: e.activation(out=l1T[:, 0:128], in_=Ga[0:64, 0:128], func=AF.Tanh), reads=[bGa], writes=[bl1T])
            op("act", lambda e, Ga=Ga: e.activation(out=l1T[:, 128:256], in_=Ga[0:64, 128:256], func=AF.Copy), reads=[bGa], writes=[bl1T])
            (Gb, bGb) = gbank()
            op("pe", lambda e, Gb=Gb: e.matmul(Gb[:, 0:256], lhsT=l1T[:, 0:128], rhs=W2[:, 0:256], start=True, stop=True), reads=[bl1T, bW2], writes=[bGb])
            op("pe", lambda e, Gb=Gb: e.matmul(Gb[:, 256:512], lhsT=l1T[:, 128:256], rhs=W2[:, 256:512], start=True, stop=True), reads=[bl1T, bW2], writes=[bGb])
            op("dve", lambda e, Gb=Gb: e.tensor_tensor(out=sgin[:], in0=Gb[:, :], in1=B01[:], op=ALU.add), reads=[bGb, bB01], writes=[bsgin])
            op("act", lambda e: e.activation(out=sga[:], in_=sgin[:], func=AF.Sigmoid), reads=[bsgin], writes=[bsga])
            (Gc, bGc) = gbank()
            (Gd, bGd) = gbank()
            op("pe", lambda e, Gc=Gc: e.matmul(Gc[:, 0:256], lhsT=TRI[:, 0:128], rhs=sga[:, 0:256], start=True, stop=True), reads=[bTRI, bsga], writes=[bGc])
            op("pe", lambda e, Gc=Gc: e.matmul(Gc[:, 256:512], lhsT=TRI[:, 128:256], rhs=sga[:, 0:256], start=True, stop=True), reads=[bTRI, bsga], writes=[bGc])
            op("pe", lambda e, Gd=Gd: e.matmul(Gd[:, 0:256], lhsT=TRI[:, 256:384], rhs=sga[:, 0:256], start=True, stop=True), reads=[bTRI, bsga], writes=[bGd])
            for h in range(4):
                op("pe", lambda e, Gd=Gd, h=h: e.matmul(Gd[0:64, 256 + 64 * h:320 + 64 * h], lhsT=sga[:, h * 64:(h + 1) * 64], rhs=CHt[:, :], start=True, stop=True),
                   reads=[bsga, bCH], writes=[bGd])
            op("act", lambda e, Gc=Gc: e.activation(out=Eg[:, 0:512], in_=Gc[:, :], func=AF.Exp, scale=-CDEC), reads=[bGc], writes=[bEg])
            op("act", lambda e, Gc=Gc: e.activation(out=gi[:], in_=Gc[:, 0:256], func=AF.Exp, scale=CDEC), reads=[bGc], writes=[bgi])
            op("act", lambda e, Gd=Gd: e.activation(out=Eg[:, 512:768], in_=Gd[:, 0:256], func=AF.Exp, scale=-CDEC), reads=[bGd], writes=[bEg])
            op("act", lambda e, Gd=Gd: e.activation(out=gLT[:].rearrange("p (h c) -> p h c", h=4), in_=Gd[0:64, 256:512].rearrange("p (h c) -> p h c", h=4)[:, :, 0:2], func=AF.Exp, scale=-CDEC), reads=[bGd], writes=[bgLT])
            if stop <= 3:
                return
            r_ = rk_t[:, 0:256]
            kr_ = rk_t[:, 256:512]
            a_ = sga[:, 256:512]
            gam, gpv, glg = Eg[:, 0:256], Eg[:, 256:512], Eg[:, 512:768]
            dv = lambda fn, reads, writes: op("dve", fn, reads=reads, writes=writes)
            dv(lambda e: e.tensor_tensor(out=tmpA[:], in0=kr_, in1=KKp, op=ALU.mult), [brk, bPV], [btmpA])
            dv(lambda e: e.tensor_tensor(out=tmpB[:], in0=tmpA[:], in1=tmpA[:], op=ALU.mult), [btmpA], [btmpB])
            dv(lambda e: e.tensor_reduce(out=s4[:, 0:4], in_=h4(tmpB[:]), axis=AX.X, op=ALU.add), [btmpB], [bs4])
            op("act", lambda e: e.activation(out=s4[:, 4:8], in_=s4[:, 0:4], func=AF.Sqrt, bias=EPS[:, 0:1], scale=1.0), reads=[bs4, bEPS], writes=[bs4])
            dv(lambda e: e.reciprocal(out=s4[:, 8:12], in_=s4[:, 4:8]), [bs4], [bs4])
            dv(lambda e: e.tensor_tensor(out=h4(kk_t[:]), in0=h4(tmpA[:]), in1=bc4(s4[:, 8:12]), op=ALU.mult), [btmpA, bs4], [bkk])
            dv(lambda e: e.scalar_tensor_tensor(out=tmpB[:], in0=a_, scalar=-1.0, in1=KAp, op0=ALU.add, op1=ALU.mult), [bsga, bPV], [btmpB])
            dv(lambda e: e.scalar_tensor_tensor(out=k_t[:], in0=tmpB[:], scalar=1.0, in1=kr_, op0=ALU.add, op1=ALU.mult), [btmpB, brk], [bk])
            dv(lambda e: e.tensor_tensor(out=ka_t[:], in0=kk_t[:], in1=a_, op=ALU.mult), [bkk, bsga], [bka])
            dv(lambda e: e.scalar_tensor_tensor(out=TA[:, 0:256], in0=kk_t[:], scalar=-1.0, in1=gpv, op0=ALU.mult, op1=ALU.mult), [bkk, bEg], [bTA])
            dv(lambda e: e.tensor_tensor(out=TA[:, 256:512], in0=r_, in1=gam, op=ALU.mult), [brk, bEg], [bTA])
            dv(lambda e: e.tensor_tensor(out=TA[:, 512:768], in0=ka_t[:], in1=gi[:], op=ALU.mult), [bka, bgi], [bTA])
            dv(lambda e: e.tensor_tensor(out=TA[:, 768:1024], in0=k_t[:], in1=gi[:], op=ALU.mult), [bk, bgi], [bTA])
            dv(lambda e: e.tensor_tensor(out=btl[:], in0=ka_t[:], in1=glg, op=ALU.mult), [bka, bEg], [bbtl])
            dv(lambda e: e.tensor_tensor(out=ktl[:], in0=k_t[:], in1=glg, op=ALU.mult), [bk, bEg], [bktl])
            dv(lambda e: e.tensor_tensor(out=tmpA[:], in0=r_, in1=k_t[:], op=ALU.mult), [brk, bk], [btmpA])
            dv(lambda e: e.tensor_tensor(out=tmpA[:], in0=tmpA[:], in1=RKp, op=ALU.mult), [bPV], [btmpA])
            dv(lambda e: e.tensor_reduce(out=s4[:, 12:16], in_=h4(tmpA[:]), axis=AX.X, op=ALU.add), [btmpA], [bs4])
            for p in range(2):
                for qn in range(4):
                    op("pe", lambda e, p=p, qn=qn: e.transpose(TBt[:, p * 512 + qn * 128:p * 512 + (qn + 1) * 128],
                                                              TA[:, qn * 256 + p * 128:qn * 256 + (p + 1) * 128], IDB[:]),
                       reads=[bTA, bIDB], writes=[bTB])
            op("act", lambda e: e.activation(out=FT[:, 0:512], in_=TBt[:, 0:512], func=AF.Copy), reads=[bTB], writes=[bFT])
            op("act", lambda e: e.activation(out=FT[:, 512:1024], in_=TBt[:, 512:1024], func=AF.Copy), reads=[bTB], writes=[bFT])
            if stop <= 4:
                return
            for h in range(4):
                p, j = h // 2, h % 2
                rows = slice(64 * j, 64 * j + 64)
                base = p * 512
                (Gx, bGx) = G[2 + j]
                (Gy, bGy) = G[j]
                op("pe", lambda e, Gx=Gx, rows=rows, base=base: e.matmul(Gx[:, 0:256], lhsT=FT[rows, base + 256:base + 384], rhs=FT[rows, base:base + 256], start=True, stop=True),
                   reads=[bFT], writes=[bGx])
                op("pe", lambda e, Gx=Gx, rows=rows, base=base: e.matmul(Gx[:, 256:512], lhsT=FT[rows, base + 384:base + 512], rhs=FT[rows, base:base + 256], start=True, stop=True),
                   reads=[bFT], writes=[bGx])
                op("pe", lambda e, Gy=Gy, rows=rows, base=base, p=p: e.matmul(Gy[:, p * 128:(p + 1) * 128], lhsT=FT[rows, base:base + 128], rhs=FT[rows, base + 256:base + 384], start=True, stop=True),
                   reads=[bFT], writes=[bGy])
                dv(lambda e, Gx=Gx, h=h: e.tensor_tensor(out=AT[h][0][:], in0=Gx[:, :], in1=MK4[:], op=ALU.mult), [bGx, bMK4], [AT[h][1]])
            for j in range(2):
                (Gy, bGy) = G[j]
                dv(lambda e, Gy=Gy, j=j: e.tensor_tensor(out=YS[0][0][:].rearrange("p (a b t) -> p a b t", a=2, b=2)[:, :, j, :],
                                                         in0=Gy[:, 0:256].rearrange("p (a t) -> p a t", a=2),
                                                         in1=MKTS[:, 0:256].rearrange("p (a t) -> p a t", a=2), op=ALU.mult), [bGy, bMKTS], [YS[0][1]])
            for h in range(4):
                op("pool", lambda e, h=h: e.tensor_copy(out=XS[0][0][:, h * 128:(h + 1) * 128], in_=AT[h][0][:, 0:128]), reads=[AT[h][1]], writes=[XS[0][1]])
            if stop <= 5:
                return
            (Gw, bGw) = gbank()
            for h in range(4):
                op("pe", lambda e, Gw=Gw, h=h: e.matmul(Gw[:, h * 64:(h + 1) * 64], lhsT=AT[h][0][:, 256:384], rhs=vbf[:, h * 64:(h + 1) * 64], start=True, stop=True),
                   reads=[AT[h][1], bvbf], writes=[bGw])
            op("act", lambda e, Gw=Gw: e.activation(out=Z[0][0][:, :, 64:128], in_=h4(Gw[:, 0:256]), func=AF.Copy), reads=[bGw], writes=[Z[0][1]])
            op("pool", lambda e: e.tensor_copy(out=Z[0][0][:, :, 0:64], in_=h4(TA[:, 0:256])), reads=[bTA], writes=[Z[0][1]])
            for lv in range(6):
                Xc, bXc = XS[lv % 2]
                Yc, bYc = YS[lv % 2]
                Xn, bXn = XS[(lv + 1) % 2]
                Yn, bYn = YS[(lv + 1) % 2]
                Zc, bZc = Z[lv % 2]
                Zn, bZn = Z[(lv + 1) % 2]
                (Gz, bGz) = gbank()
                for h in range(4):
                    op("pe", lambda e, Gz=Gz, h=h, Xc=Xc, Zc=Zc: e.matmul(Gz[:, h * 128:(h + 1) * 128], lhsT=Xc[:, h * 128:(h + 1) * 128], rhs=Zc[:, h, :], start=True, stop=False),
                       reads=[bXc, bZc], writes=[bGz])
                    op("pe", lambda e, Gz=Gz, h=h, Zc=Zc: e.matmul(Gz[:, h * 128:(h + 1) * 128], lhsT=IDB[:], rhs=Zc[:, h, :], start=False, stop=True),
                       reads=[bIDB, bZc], writes=[bGz])
                op("act", lambda e, Gz=Gz, Zn=Zn: e.activation(out=Zn[:].rearrange("p h n -> p (h n)"), in_=Gz[:, :], func=AF.Copy), reads=[bGz], writes=[bZn])
                if lv < 5:
                    (G1, bG1) = gbank()
                    (G2, bG2) = gbank()
                    for h in range(4):
                        hs = slice(h * 128, (h + 1) * 128)
                        op("pe", lambda e, G1=G1, hs=hs, Xc=Xc, Yc=Yc: e.matmul(G1[:, hs], lhsT=Yc[:, hs], rhs=Xc[:, hs], start=True, stop=True), reads=[bXc, bYc], writes=[bG1])
                        op("pe", lambda e, G2=G2, hs=hs, Xc=Xc, Yc=Yc: e.matmul(G2[:, hs], lhsT=Xc[:, hs], rhs=Yc[:, hs], start=True, stop=True), reads=[bXc, bYc], writes=[bG2])
                    dv(lambda e, G1=G1, Xn=Xn: e.tensor_copy(out=Xn[:], in_=G1[:, :]), [bG1], [bXn])
                    op("act", lambda e, G2=G2, Yn=Yn: e.activation(out=Yn[:], in_=G2[:, :], func=AF.Copy), reads=[bG2], writes=[bYn])
            if stop <= 6:
                return
            Zf, bZf = Z[0]
            for c in range(2):
                rows = slice(64 * c, 64 * c + 64)
                (Gp, bGp) = gbank()
                for h in range(4):
                    hc = slice(h * 64, (h + 1) * 64)
                    op("pe", lambda e, Gp=Gp, h=h, hc=hc, rows=rows: e.matmul(Gp[0:64, h * 128:h * 128 + 64], lhsT=Zf[rows, h, 0:64], rhs=btl[rows, hc], start=True, stop=True),
                       reads=[bZf, bbtl], writes=[bGp])
                    op("pe", lambda e, Gp=Gp, h=h, hc=hc, rows=rows: e.matmul(Gp[0:64, h * 128 + 64:h * 128 + 128], lhsT=btl[rows, hc], rhs=Zf[rows, h, 64:128], start=True, stop=False),
                       reads=[bZf, bbtl], writes=[bGp])
                    op("pe", lambda e, Gp=Gp, h=h, hc=hc, rows=rows: e.matmul(Gp[0:64, h * 128 + 64:h * 128 + 128], lhsT=ktl[rows, hc], rhs=vbf[rows, hc], start=False, stop=True),
                       reads=[bktl, bvbf], writes=[bGp])
                dv(lambda e, c=c: e.tensor_tensor(out=DG[:, :, 0:64], in0=IDF[0:64, 0:64].unsqueeze(1).to_broadcast([64, 4, 64]),
                                                  in1=gLT[:, c:8:2].unsqueeze(2).to_broadcast([64, 4, 64]), op=ALU.mult), [bIDF, bgLT], [bDG])
                dv(lambda e, c=c, Gp=Gp: e.tensor_tensor(out=PQs[:, c, :, :], in0=Gp[0:64, :].rearrange("p (h n) -> p h n", h=4), in1=DG[:], op=ALU.add), [bGp, bDG], [bPQs])
            (Gr, bGr) = gbank()
            for h in range(4):
                op("pe", lambda e, Gr=Gr, h=h: e.matmul(Gr[0:64, h * 128:(h + 1) * 128], lhsT=Zf[:, h, 0:64], rhs=AT[h][0][:, 128:256], start=True, stop=False),
                   reads=[bZf, AT[h][1]], writes=[bGr])
                op("pe", lambda e, Gr=Gr, h=h: e.matmul(Gr[0:64, h * 128:(h + 1) * 128], lhsT=TA[:, 256 + h * 64:256 + (h + 1) * 64], rhs=IDB[:], start=False, stop=True),
                   reads=[bTA, bIDB], writes=[bGr])
            op("act", lambda e, Gr=Gr: e.activation(out=RP[0][0][:, :, 0:64], in_=Gr[0:64, :].rearrange("p (h n) -> p h n", h=4)[:, :, 0:64], func=AF.Copy), reads=[bGr], writes=[RP[0][1]])
            op("act", lambda e, Gr=Gr: e.activation(out=RP[1][0][:, :, 64:128], in_=Gr[0:64, :].rearrange("p (h n) -> p h n", h=4)[:, :, 64:128], func=AF.Copy), reads=[bGr], writes=[RP[1][1]])
            for c in range(2):
                Tc, bTc = TS[c]
                Tn, bTn = TS[(c + 1) % 2]
                (Gs, bGs_) = gbank()
                for h in range(4):
                    op("pe", lambda e, Gs=Gs, h=h, c=c, Tc=Tc: e.matmul(Gs[0:64, h * 64:(h + 1) * 64], lhsT=PQs[:, c, h, 0:64], rhs=Tc[:, h, :], start=True, stop=False),
                       reads=[bPQs, bTc], writes=[bGs_])
                    op("pe", lambda e, Gs=Gs, h=h, c=c: e.matmul(Gs[0:64, h * 64:(h + 1) * 64], lhsT=IDF[0:64, 0:64], rhs=PQs[:, c, h, 64:128], start=False, stop=True),
                       reads=[bPQs, bIDF], writes=[bGs_])
                if c == 0:
                    op("act", lambda e, Gs=Gs: e.activation(out=TS[1][0][:].rearrange("p h n -> p (h n)"), in_=Gs[0:64, 0:256], func=AF.Copy), reads=[bGs_], writes=[TS[1][1]])
                    dv(lambda e: e.tensor_copy(out=TSb[1][0][:], in_=TS[1][0][:]), [TS[1][1]], [TSb[1][1]])
            (Gq, bGq) = gbank()
            if Gq is Gs:
                (Gq, bGq) = gbank()
            for h in range(4):
                hc = slice(h * 64, (h + 1) * 64)
                op("pe", lambda e, Gq=Gq, h=h, hc=hc: e.matmul(Gq[:, hc], lhsT=AT[h][0][:, 128:256], rhs=Zf[:, h, 64:128], start=True, stop=False), reads=[AT[h][1], bZf], writes=[bGq])
                op("pe", lambda e, Gq=Gq, h=h, hc=hc: e.matmul(Gq[:, hc], lhsT=AT[h][0][:, 384:512], rhs=vbf[:, hc], start=False, stop=False), reads=[AT[h][1], bvbf], writes=[bGq])
                op("pe", lambda e, Gq=Gq, h=h, hc=hc: e.matmul(Gq[:, hc], lhsT=RP[0][0][:, h, :], rhs=TSb[0][0][:, h, :], start=False, stop=False), reads=[RP[0][1], TSb[0][1]], writes=[bGq])
                op("pe", lambda e, Gq=Gq, h=h, hc=hc: e.matmul(Gq[:, hc], lhsT=RP[1][0][:, h, :], rhs=TSb[1][0][:, h, :], start=False, stop=True), reads=[RP[1][1], TSb[1][1]], writes=[bGq])
            op("act", lambda e, Gs=Gs: e.activation(out=TS[0][0][:].rearrange("p h n -> p (h n)"), in_=Gs[0:64, 0:256], func=AF.Copy), reads=[bGs_], writes=[TS[0][1]])
            dv(lambda e: e.tensor_copy(out=TSb[0][0][:], in_=TS[0][0][:]), [TS[0][1]], [TSb[0][1]])
            if stop <= 7:
                return
            op("act", lambda e, Gq=Gq: e.activation(out=Yt[:], in_=Gq[:, 0:256], func=AF.Copy), reads=[bGq], writes=[bYt])
            dv(lambda e: e.tensor_reduce(out=s4[:, 0:4], in_=h4(Yt[:]), axis=AX.X, op=ALU.add), [bYt], [bs4])
            dv(lambda e: e.tensor_tensor(out=Ysq[:], in0=Yt[:], in1=Yt[:], op=ALU.mult), [bYt], [bYsq])
            dv(lambda e: e.tensor_reduce(out=s4[:, 4:8], in_=h4(Ysq[:]), axis=AX.X, op=ALU.add), [bYsq], [bs4])
            dv(lambda e: e.tensor_scalar(out=s4[:, 0:4], in0=s4[:, 0:4], scalar1=1.0 / 64, scalar2=None, op0=ALU.mult), [bs4], [bs4])
            dv(lambda e: e.tensor_tensor(out=s4[:, 8:12], in0=s4[:, 0:4], in1=s4[:, 0:4], op=ALU.mult), [bs4], [bs4])
            dv(lambda e: e.scalar_tensor_tensor(out=s4[:, 4:8], in0=s4[:, 4:8], scalar=1.0 / 64, in1=s4[:, 8:12], op0=ALU.mult, op1=ALU.subtract), [bs4], [bs4])
            op("act", lambda e: e.activation(out=s4[:, 8:12], in_=s4[:, 4:8], func=AF.Sqrt, bias=EPS[:, 1:2], scale=1.0), reads=[bs4, bEPS], writes=[bs4])
            dv(lambda e: e.reciprocal(out=s4[:, 4:8], in_=s4[:, 8:12]), [bs4], [bs4])
            dv(lambda e: e.tensor_tensor(out=h4(Yt[:]), in0=h4(Yt[:]), in1=bc4(s4[:, 0:4]), op=ALU.subtract), [bs4], [bYt])
            dv(lambda e: e.tensor_tensor(out=h4(Yt[:]), in0=h4(Yt[:]), in1=bc4(s4[:, 4:8]), op=ALU.mult), [bs4], [bYt])
            dv(lambda e: e.tensor_tensor(out=Yt[:], in0=Yt[:], in1=LNG, op=ALU.mult), [bPV], [bYt])
            dv(lambda e: e.tensor_tensor(out=Yt[:], in0=Yt[:], in1=LNB, op=ALU.add), [bPV], [bYt])
            dv(lambda e: e.tensor_tensor(out=h4(Ysq[:]), in0=h4(v_t[:]), in1=bc4(s4[:, 12:16]), op=ALU.mult), [bv, bs4], [bYsq])
            dv(lambda e: e.tensor_tensor(out=Yt[:], in0=Yt[:], in1=Ysq[:], op=ALU.add), [bYsq], [bYt])
            dv(lambda e: e.tensor_tensor(out=yrb[:], in0=Yt[:], in1=szr[:], op=ALU.mult), [bYt, bszr], [byrb])

            if stop <= 8:
                return
            for p in range(2):
                op("pe", lambda e, p=p: e.transpose(TBt[:, p * 128:(p + 1) * 128], qkb[:, p * 128:(p + 1) * 128], IDB[:]), reads=[bqkb, bIDB], writes=[bTB])
                op("pe", lambda e, p=p: e.transpose(TBt[:, 256 + p * 128:256 + (p + 1) * 128], qkb[:, 256 + p * 128:256 + (p + 1) * 128], IDB[:]), reads=[bqkb, bIDB], writes=[bTB])
            op("act", lambda e: e.activation(out=qT[:].rearrange("p a t -> p (a t)"), in_=TBt[:, 0:256], func=AF.Copy), reads=[bTB], writes=[bqT])
            op("act", lambda e, slot=slot: e.activation(out=kTr[:, :, slot, :], in_=TBt[:, 256:512].rearrange("p (a t) -> p a t", a=2), func=AF.Copy), reads=[bTB], writes=[bkr[slot]])
            m0 = max(0, 4 - blk)
            (Go, bGo) = gbank()
            for h in range(4):
                p, j = h // 2, h % 2
                rows = slice(64 * j, 64 * j + 64)
                PTh, bPTh = PT[h % 2]
                (Gs1, bGs1) = gbank()
                if Gs1 is Go:
                    (Gs1, bGs1) = gbank()
                (Gs2, bGs2) = gbank()
                if Gs2 is Go:
                    (Gs2, bGs2) = gbank()
                for m in range(m0, 5):
                    kb = blk - 4 + m
                    sl = kb % NR
                    Gt, bGt, cc = (Gs1, bGs1, m * 128) if m < 4 else (Gs2, bGs2, 0)
                    op("pe", lambda e, Gt=Gt, cc=cc, rows=rows, p=p, sl=sl: e.matmul(Gt[:, cc:cc + 128], lhsT=kTr[rows, p, sl, :], rhs=qT[rows, p, :], start=True, stop=False),
                       reads=[bkr[sl], bqT], writes=[bGt])
                    op("pe", lambda e, Gt=Gt, cc=cc, h=h, m=m: e.matmul(Gt[:, cc:cc + 128], lhsT=IDB[:], rhs=BM[:, h * 640 + m * 128:h * 640 + (m + 1) * 128], start=False, stop=True),
                       reads=[bIDB, bBM], writes=[bGt])
                if m0 < 4:
                    op("act", lambda e, Gs1=Gs1, PTh=PTh, m0=m0: e.activation(out=PTh[:, m0:4, :], in_=Gs1[:, m0 * 128:512].rearrange("p (m t) -> p m t", t=128), func=AF.Exp),
                       reads=[bGs1], writes=[bPTh])
                op("act", lambda e, Gs2=Gs2, PTh=PTh: e.activation(out=PTh[:, 4, :], in_=Gs2[:, 0:128], func=AF.Exp), reads=[bGs2], writes=[bPTh])
                for m in range(m0, 5):
                    kb = blk - 4 + m
                    sl = kb % NR
                    op("pe", lambda e, Go=Go, h=h, m=m, sl=sl, PTh=PTh: e.matmul(Go[:, h * 65:h * 65 + 64], lhsT=PTh[:, m, :], rhs=vring[:, sl, h * 64:(h + 1) * 64], start=(m == m0), stop=(m == 4)),
                       reads=[bPTh, bvr[sl]], writes=[bGo])

                for m in range(m0, 5):
                    op("pe", lambda e, Go=Go, h=h, m=m, PTh=PTh: e.matmul(Go[:, h * 65 + 64:h * 65 + 65], lhsT=PTh[:, m, :], rhs=ONEC[:, 0:1], start=(m == m0), stop=(m == 4)),
                       reads=[bPTh, bONEC], writes=[bGo])
            Go3 = Go[:, 0:260].rearrange("p (h n) -> p h n", h=4)
            dv(lambda e, Go3=Go3: e.reciprocal(out=s4[:, 0:4], in_=Go3[:, :, 64]), [bGo], [bs4])
            dv(lambda e, Go3=Go3: e.tensor_tensor(out=h4(ya_t[:]), in0=Go3[:, :, 0:64], in1=bc4(s4[:, 0:4]), op=ALU.mult), [bGo, bs4], [bya])
            dv(lambda e: e.tensor_tensor(out=yab[:], in0=ya_t[:], in1=sza[:], op=ALU.mult), [bya, bsza], [byab])
            if stop <= 9:
                return
            yT_s, byT = yTs[blk % 2]
            for p in range(2):
                op("pe", lambda e, p=p: e.transpose(TBt[:, p * 128:(p + 1) * 128], yrb[:, p * 128:(p + 1) * 128], IDB[:]), reads=[byrb, bIDB], writes=[bTB])
                op("pe", lambda e, p=p: e.transpose(TBt[:, 256 + p * 128:256 + (p + 1) * 128], yab[:, p * 128:(p + 1) * 128], IDB[:]), reads=[byab, bIDB], writes=[bTB])
            op("act", lambda e, yT_s=yT_s: e.activation(out=yT_s[:].rearrange("p a t -> p (a t)"), in_=TBt[:, 0:512], func=AF.Copy), reads=[bTB], writes=[byT])
            op("pool", lambda e, yT_s=yT_s, blk=blk: e.dma_start(out=yT_d[:, blk * 128:(blk + 1) * 128].rearrange("(a p) t -> p a t", p=128), in_=yT_s[:]),
               reads=[byT], dma="yo%d" % (blk % 2))

        for blk_ in range(nblk):
            do_block(blk_)
        if os.environ.get("K_DBG") == "1":
            dl = [("xs", xs, bxs, [128, 2048], BF16), ("hA", hT[0][0], hT[0][1], [128, 16, 128], BF16), ("hB", hBt, bhB, [128, 16, 128], BF16),
                  ("rk", rk_t, brk, [128, 512], F32), ("v", v_t, bv, [128, 256], F32), ("l1T", l1T, bl1T, [64, 256], F32), ("sga", sga, bsga, [128, 512], F32),
                  ("Eg", Eg, bEg, [128, 768], F32), ("gi", gi, bgi, [128, 256], F32), ("gLT", gLT, bgLT, [64, 8], F32), ("szr", szr, bszr, [128, 256], F32),
                  ("sza", sza, bsza, [128, 256], F32), ("qkb", qkb, bqkb, [128, 512], BF16), ("TA", TA, bTA, [128, 1024], BF16), ("FT", FT, bFT, [128, 1024], BF16),
                  ("AT0", AT[0][0], AT[0][1], [128, 512], BF16), ("AT1", AT[1][0], AT[1][1], [128, 512], BF16), ("Z0", Z[0][0], Z[0][1], [128, 4, 128], BF16),
                  ("PQs", PQs, bPQs, [64, 2, 4, 128], F32), ("RP0", RP[0][0], RP[0][1], [64, 4, 128], BF16), ("TS0", TS[0][0], TS[0][1], [64, 4, 64], F32),
                  ("Yt", Yt, bYt, [128, 256], F32), ("yrb", yrb, byrb, [128, 256], BF16), ("qT", qT, bqT, [128, 2, 128], BF16),
                  ("ya", ya_t, bya, [128, 256], F32), ("yab", yab, byab, [128, 256], BF16),
                  ("WA0", WA, bWA, [128, 16, 2176], BF16), ("XS0", XS[0][0], XS[0][1], [128, 512], BF16),
                  ("YS0", YS[0][0], YS[0][1], [128, 512], BF16), ("BM", BM, bBM, [128, 2560], BF16)]
            for (nm, t, bb, shp, dt_) in dl:
                dd = dr("dbg_" + nm, shp, dt_, kind="ExternalOutput")
                extra = bkr + bvr if nm in ("kTr", "vring") else []
                op("sp", lambda e, dd=dd, t=t: e.dma_start(out=dd, in_=t[:]), reads=[bb] + extra, dma="yo_dbg")

        for name, (sem, cnt) in cx.dma.items():
            if name.startswith("yo"):
                cx.wait_tok("sp", ("dma:" + name, sem, cnt))
        cx.emit()
    return nc


def _consts():
    idx = np.arange(128)
    same = (idx[:, None] // 64) == (idx[None, :] // 64)
    incl = ((idx[:, None] <= idx[None, :]) & same).astype(np.float32)
    excl = ((idx[:, None] < idx[None, :]) & same).astype(np.float32)
    suf = ((idx[:, None] > idx[None, :]) & same).astype(np.float32)
    c = {}
    c["ident"] = np.eye(128, dtype=np.float32)
    c["tri3"] = np.ascontiguousarray(np.concatenate([incl, excl, suf], 1))
    c["mask4"] = np.ascontiguousarray(np.concatenate([excl, incl, excl, incl], 1))
    c["maskts4"] = np.ascontiguousarray(np.concatenate([suf] * 4, 1))
    c["ch"] = np.zeros((128, 64), np.float32)
    c["ch"][0:64, 0] = 1.0
    c["ch"][64:128, 1] = 1.0
    mA = np.zeros((128, 5, 128), np.float32)
    mA[0:64, 0, 64:128] = NEG
    mA[64:128, 4, 0:64] = NEG
    c["maskA"] = np.ascontiguousarray(mA.reshape(128, 640))
    kk = idx[:, None, None]
    m = np.arange(5)[None, :, None]
    qq = idx[None, None, :]
    rel = 128 * (4 - m) + qq - kk
    c["relidx"] = np.clip(rel, -256, 256) + 256
    return c


def _p1_inputs(inp, core):
    b, g = core // 4, core % 4
    c = _consts()
    w_in = inp["w_in"][0]
    hs = slice(g * 256, (g + 1) * 256)
    col = lambda i: w_in[:, i * 1024 + g * 256: i * 1024 + (g + 1) * 256]
    wcat = np.concatenate([col(0), col(1), col(2), inp["w1"][0], inp["a1"][0], col(3), col(6), col(7), col(4), col(5)], axis=1)
    m = {}
    m["xb"] = np.ascontiguousarray(inp["x"][b])
    m["w1p"] = np.ascontiguousarray(wcat.reshape(16, 128, 2176).transpose(1, 0, 2))
    m["w2a2"] = np.ascontiguousarray(np.concatenate([inp["w2"][0][:, hs], inp["a2"][0][:, hs]], 1))
    m["bias01"] = np.ascontiguousarray(np.concatenate([inp["w0"][0][hs], inp["a0"][0][hs]])[None, :])
    pv = [inp["mu_rkv"][0][0][hs], inp["mu_rkv"][0][1][hs], inp["mu_rkv"][0][2][hs], inp["k_k"][0][hs], inp["k_a"][0][hs],
          inp["r_k"][0].reshape(-1)[hs], inp["ln_x_g"][0][hs], inp["ln_x_b"][0][hs]]
    m["pvec"] = np.ascontiguousarray(np.concatenate(pv)[None, :])
    pc = [inp["pre_norm_g"][0].reshape(16, 128).T, inp["mu_wa"][0][0].reshape(16, 128).T, inp["mu_wa"][0][1].reshape(16, 128).T]
    m["pcol"] = np.ascontiguousarray(np.concatenate(pc, 1))
    for k in ["ident", "tri3", "mask4", "maskts4", "ch", "maskA"]:
        m[k] = c[k]
    rb = inp["rel_bias"][0][g * 4:(g + 1) * 4]
    bt = rb[:, c["relidx"]]
    m["biasT"] = np.ascontiguousarray(bt.transpose(1, 0, 2, 3).reshape(128, 4 * 640))
    return {k: np.asarray(v, dtype=np.float32) for k, v in m.items()}


def kernel(**inputs):
    inp = {k: np.asarray(v) for k, v in inputs.items()}
    nc1 = build("p1")
    in_maps = [_p1_inputs(inp, c) for c in range(8)]
    res1 = run_bass_kernel_spmd(nc1, in_maps, core_ids=list(range(8)))
    yT = [np.asarray(res1.results[c]["yT"]) for c in range(8)]
    nc2 = build_p2()
    in_maps2 = [_p2_inputs(inp, c, yT) for c in range(8)]
    res2 = run_bass_kernel_spmd(nc2, in_maps2, core_ids=list(range(8)))
    out = np.empty((2, S, D), np.float32)
    for c in range(8):
        b_, g_ = c // 4, c % 4
        out[b_, g_ * 2048:(g_ + 1) * 2048] = np.asarray(res2.results[c]["out"])
    return out


TT = 512


def build_p2(n_tt=4, stop=99):
    nc = bass.Bass("TRN2", target_bir_lowering=False)
    dr = lambda name, shape, dt=F32, kind="ExternalInput": nc.dram_tensor(name, shape, dt, kind=kind).ap()
    xq = dr("xq", [2048, D])
    yTall = dr("yTall", [2048, 2048], mybir.dt.uint16).bitcast(BF16)
    wg = dr("wg", [16, 2, 128, 2048])
    wb = dr("wb", [16, 128, 2048])
    wout = dr("wout", [128, 16, 2048])
    bmc = dr("bmc", [128, 32])
    gpost = dr("gpost", [1, 2048])
    pcol = dr("pcol2", [128, 16])
    ident_d = dr("ident", [128, 128])
    out_d = dr("out", [2048, D], kind="ExternalOutput")
    with ExitStack() as st:
        cx = Ctx(nc, st)
        phase2_body(nc, cx, st, xq, yTall, wg, wb, wout, bmc, gpost, pcol, ident_d, out_d, n_tt, stop)
        for name, (sem, cnt) in cx.dma.items():
            if name.startswith("oo"):
                cx.wait_tok("sp", ("dma:" + name, sem, cnt))
        cx.emit()
    return nc


def phase2_body(nc, cx, st, xq, yTall, wg, wb, wout, bmc, gpost, pcol, ident_d, out_d, n_tt, stop=99):
    op = cx.op

    def sb(name, shape, dt=F32):
        t = st.enter_context(nc.sbuf_tensor(name, shape, dt))
        return t, Buf(name)

    def ps(name, shape, dt=F32):
        t = st.enter_context(nc.psum_tensor(name, shape, dt))
        return t, Buf(name)

    def ld(eng, dst, bdst, src, name):
        op(eng, lambda e: e.dma_start(out=dst, in_=src), writes=[bdst], dma=name)

    SCRd, _ = sb("SCRd2", [128, 256])
    SCRa, _ = sb("SCRa2", [128, 256])
    cx.spacer["dve"] = lambda e: e.memset(SCRd[:], 0.0)
    cx.spacer["act"] = lambda e: e.activation(out=SCRa[:], in_=SCRd[:], func=AF.Copy)
    op("dve", lambda e: e.memset(SCRd[:], 0.0))
    WO, bWO = sb("WO", [128, 16, 2048], BF16)
    GP, bGP = sb("GP", [128, 2048])
    BMc, bBMc = sb("BMc", [128, 32])
    PCg, bPCg = sb("PCg", [128, 16])
    IDF, bIDF = sb("IDF2", [128, 128])
    IDB, bIDB = sb("IDB2", [128, 128], BF16)
    EPS, bEPS = sb("EPS2", [128, 1])
    hT2, bhT2 = sb("hT2", [128, 16, TT], BF16)
    yT2, byT2 = sb("yT2", [128, 16, TT], BF16)
    mT, bmT = sb("mT", [128, 16, TT], BF16)
    wst = [sb("wst2_%d" % i, [128, 2048]) for i in range(2)]
    wgb = [sb("wgb%d" % i, [128, 2, 2048], BF16) for i in range(2)]
    wbb = [sb("wbb%d" % i, [128, 2048], BF16) for i in range(2)]
    xt, bxt = sb("xt2", [128, D])
    xs, bxs = sb("xs2", [128, D], BF16)
    ot, bot = sb("ot2", [128, D])
    st8, bst8 = sb("st8_2", [128, 8])
    sr, bsr = sb("sr", [128, TT])
    sa, bsa = sb("sa", [128, TT])
    t1, bt1 = sb("t1", [128, TT])
    t2, bt2 = sb("t2", [128, TT])
    Q = [ps("Q%d" % i, [128, 512]) for i in range(4)]
    QT, bQT = ps("QT", [128, 1024], BF16)

    ld("sp", GP[:], bGP, gpost[0:1, :].partition_broadcast(128), "k0")
    ld("sp", BMc[:], bBMc, bmc[:, :], "k1")
    ld("sp", PCg[:], bPCg, pcol[:, :], "k2")
    ld("sp", IDF[:], bIDF, ident_d[:, :], "k3")
    op("dve", lambda e: e.tensor_copy(out=IDB[:], in_=IDF[:]), reads=[bIDF], writes=[bIDB])
    op("dve", lambda e: e.memset(EPS[:], 1e-6), writes=[bEPS])
    for kt in range(16):
        wt, bwt = wst[kt % 2]
        ld("sp" if kt % 2 == 0 else "pool", wt[:], bwt, wout[:, kt, :], "ws%d" % (kt % 2))
        if kt % 2 == 0:
            op("act", lambda e, wt=wt, kt=kt: e.activation(out=WO[:, kt, :], in_=wt[:], func=AF.Copy), reads=[bwt], writes=[bWO])
        else:
            op("dve", lambda e, wt=wt, kt=kt: e.tensor_copy(out=WO[:, kt, :], in_=wt[:]), reads=[bwt], writes=[bWO])

    def norm_block(r0):
        ld("sp", xt[:], bxt, xq[r0:r0 + 128, :], "x2")
        op("dve", lambda e: e.memset(st8[:, 0:1], 0.0), writes=[bst8])
        op("act", lambda e: e.activation(out=xs[:], in_=xt[:], func=AF.Square, accum_out=st8[:, 0:1]), reads=[bxt], writes=[bxs, bst8])
        op("act", lambda e: e.activation(out=st8[:, 1:2], in_=st8[:, 0:1], func=AF.Sqrt, bias=EPS[:, 0:1], scale=1.0 / D), reads=[bEPS, bst8], writes=[bst8])
        op("dve", lambda e: e.reciprocal(out=st8[:, 2:3], in_=st8[:, 1:2]), reads=[bst8], writes=[bst8])

    def do_tile(tt):
        tok0 = tt * TT
        if stop <= 0:
            return
        for sub in range(4):
            norm_block(tok0 + sub * 128)
            op("dve", lambda e: e.tensor_scalar(out=xs[:], in0=xt[:], scalar1=st8[:, 2:3], scalar2=None, op0=ALU.mult), reads=[bxt, bst8], writes=[bxs])
            for half in range(2):
                for j in range(8):
                    kt = half * 8 + j
                    op("pe", lambda e, j=j, kt=kt: e.transpose(QT[:, j * 128:(j + 1) * 128], xs[:, kt * 128:(kt + 1) * 128], IDB[:]), reads=[bxs, bIDB], writes=[bQT])
                if half == 0:
                    op("act", lambda e, half=half, sub=sub: e.activation(out=hT2[:, half * 8:(half + 1) * 8, sub * 128:(sub + 1) * 128],
                                                                         in_=QT[:].rearrange("p (k t) -> p k t", k=8), func=AF.Copy), reads=[bQT], writes=[bhT2])
                else:
                    op("dve", lambda e, half=half, sub=sub: e.tensor_copy(out=hT2[:, half * 8:(half + 1) * 8, sub * 128:(sub + 1) * 128],
                                                                          in_=QT[:].rearrange("p (k t) -> p k t", k=8)), reads=[bQT], writes=[bhT2])
        if stop <= 1:
            return
        for br in range(2):
            for rk in range(4):
                r0 = rk * 512 + br * 256
                ld("pool", yT2[:, br * 8 + rk * 2:br * 8 + rk * 2 + 2, :], byT2,
                   yTall[r0:r0 + 256, tok0:tok0 + TT].rearrange("(a p) t -> p a t", p=128), "y2")
        if stop <= 2:
            return
        for j in range(16):
            if stop <= 3 and j >= 1:
                break
            wgt, bwg = wgb[j % 2]
            wbt, bwb = wbb[j % 2]
            for ra in range(2):
                wt, bwt = wst[ra]
                ld("sp", wt[:], bwt, wg[j, ra, :, :], "ws%d" % ra)
                op("dve",
                   lambda e, wt=wt, wgt=wgt, ra=ra: e.tensor_tensor(out=wgt[:, ra, :].rearrange("p (k c) -> p k c", k=16), in0=wt[:].rearrange("p (k c) -> p k c", k=16),
                                                                    in1=PCg[:].unsqueeze(2).to_broadcast([128, 16, 128]), op=ALU.mult),
                   reads=[bwt, bPCg], writes=[bwg])
            wt, bwt = wst[0]
            ld("pool", wt[:], bwt, wb[j, :, :], "ws0")
            op("act", lambda e, wt=wt, wbt=wbt: e.activation(out=wbt[:], in_=wt[:], func=AF.Copy), reads=[bwt], writes=[bwb])
            for ra in range(2):
                Qx, bQx = Q[ra]
                for kt in range(16):
                    op("pe", lambda e, Qx=Qx, ra=ra, kt=kt, wgt=wgt: e.matmul(Qx[:, :], lhsT=wgt[:, ra, kt * 128:(kt + 1) * 128], rhs=hT2[:, kt, :], start=(kt == 0), stop=(kt == 15)),
                       reads=[bwg, bhT2], writes=[bQx])
            for br in range(2):
                Qx, bQx = Q[2 + br]
                for kt in range(8):
                    op("pe", lambda e, Qx=Qx, br=br, kt=kt, wbt=wbt: e.matmul(Qx[:, :], lhsT=wbt[:, (br * 8 + kt) * 128:(br * 8 + kt + 1) * 128], rhs=yT2[:, br * 8 + kt, :], start=(kt == 0), stop=(kt == 7)),
                       reads=[bwb, byT2], writes=[bQx])
            op("act", lambda e, j=j: e.activation(out=sr[:], in_=Q[0][0][:, :], func=AF.Sigmoid, bias=BMc[:, j:j + 1], scale=1.0), reads=[Q[0][1], bBMc], writes=[bsr])
            op("act", lambda e, j=j: e.activation(out=sa[:], in_=Q[1][0][:, :], func=AF.Sigmoid, bias=BMc[:, 16 + j:17 + j], scale=1.0), reads=[Q[1][1], bBMc], writes=[bsa])
            op("dve", lambda e: e.tensor_tensor(out=t1[:], in0=sr[:], in1=Q[2][0][:, :], op=ALU.mult), reads=[bsr, Q[2][1]], writes=[bt1])
            op("dve", lambda e: e.tensor_tensor(out=t2[:], in0=sa[:], in1=Q[3][0][:, :], op=ALU.mult), reads=[bsa, Q[3][1]], writes=[bt2])
            op("dve", lambda e, j=j: e.tensor_tensor(out=mT[:, j, :], in0=t1[:], in1=t2[:], op=ALU.add), reads=[bt1, bt2], writes=[bmT])
        if stop <= 4:
            return
        for sub in range(4):
            r0 = tok0 + sub * 128
            for cb in range(4):
                Qx, bQx = Q[cb]
                for kt in range(16):
                    op("pe", lambda e, Qx=Qx, cb=cb, kt=kt, sub=sub: e.matmul(Qx[:, :], lhsT=mT[:, kt, sub * 128:(sub + 1) * 128], rhs=WO[:, kt, cb * 512:(cb + 1) * 512], start=(kt == 0), stop=(kt == 15)),
                       reads=[bmT, bWO], writes=[bQx])
            op("dve", lambda e: e.memset(st8[:, 4:8], 0.0), writes=[bst8])
            for cb in range(4):
                op("act", lambda e, cb=cb: e.activation(out=xs[:, cb * 512:(cb + 1) * 512], in_=Q[cb][0][:, :], func=AF.Square, accum_out=st8[:, 4 + cb:5 + cb]),
                   reads=[Q[cb][1]], writes=[bxs, bst8])
            op("dve", lambda e: e.tensor_reduce(out=st8[:, 0:1], in_=st8[:, 4:8], axis=AX.X, op=ALU.add), reads=[bst8], writes=[bst8])
            op("act", lambda e: e.activation(out=st8[:, 1:2], in_=st8[:, 0:1], func=AF.Sqrt, bias=EPS[:, 0:1], scale=1.0 / D), reads=[bEPS, bst8], writes=[bst8])
            op("dve", lambda e: e.reciprocal(out=st8[:, 2:3], in_=st8[:, 1:2]), reads=[bst8], writes=[bst8])
            ld("sp", xt[:], bxt, xq[r0:r0 + 128, :], "x2")
            for cb in range(4):
                cs = slice(cb * 512, (cb + 1) * 512)
                op("dve", lambda e, cb=cb, cs=cs: e.scalar_tensor_tensor(out=ot[:, cs], in0=Q[cb][0][:, :], scalar=st8[:, 2:3], in1=GP[:, cs], op0=ALU.mult, op1=ALU.mult),
                   reads=[Q[cb][1], bst8, bGP], writes=[bot])
            op("dve", lambda e: e.tensor_tensor(out=ot[:], in0=ot[:], in1=xt[:], op=ALU.add), reads=[bxt], writes=[bot])
            op("sp", lambda e, r0=r0: e.dma_start(out=out_d[r0:r0 + 128, :], in_=ot[:]), reads=[bot], dma="oo")

    for tt in range(n_tt):
        do_tile(tt)


def _p2_inputs(inp, core, yT_by_core):
    b, g = core // 4, core % 4
    w_in = inp["w_in"][0]
    m = {}
    m["xq"] = np.ascontiguousarray(inp["x"][b, g * 2048:(g + 1) * 2048])
    m["yTall"] = np.ascontiguousarray(np.concatenate([yT_by_core[b * 4 + r][:, g * 2048:(g + 1) * 2048] for r in range(4)], 0)).view(np.uint16)
    gates = w_in[:, 8192:12288].reshape(16, 128, 2, 16, 128)
    m["wg"] = np.ascontiguousarray(gates.transpose(3, 2, 1, 0, 4).reshape(16, 2, 128, 2048))
    wbr = inp["w_branch_rwkv"][0].reshape(8, 128, 16, 128)
    wba = inp["w_branch_attn"][0].reshape(8, 128, 16, 128)
    wbcat = np.stack([wbr, wba], 0)
    m["wb"] = np.ascontiguousarray(wbcat.transpose(3, 2, 0, 1, 4).reshape(16, 128, 2048))
    m["wout"] = np.ascontiguousarray(inp["w_out"][0].reshape(16, 128, 2048).transpose(1, 0, 2))
    bm = inp["b_merge"][0].reshape(2, 16, 128)
    m["bmc"] = np.ascontiguousarray(bm.transpose(2, 0, 1).reshape(128, 32))
    m["gpost"] = np.ascontiguousarray(inp["post_norm_g"][0][None, :])
    m["pcol2"] = np.ascontiguousarray(inp["pre_norm_g"][0].reshape(16, 128).T)
    m["ident"] = np.eye(128, dtype=np.float32)
    return m
```

```python
import os
import numpy as np
from contextlib import ExitStack
import concourse.bass as bass
import concourse.mybir as mybir
from concourse.bass_utils import run_bass_kernel_spmd

F32 = mybir.dt.float32
BF16 = mybir.dt.bfloat16
ALU = mybir.AluOpType
AF = mybir.ActivationFunctionType
AX = mybir.AxisListType

D = 2048
S = 8192
NB1 = S // 128
CDEC = 0.606531
NR = 6
NEG = -30000.0
GAP = 3
SELF_SYNC = os.environ.get("K_SELF_SYNC") == "1"


class Buf:
    __slots__ = ("name", "w", "r")

    def __init__(self, name):
        self.name = name
        self.w = None
        self.r = {}


class Ctx:
    def __init__(self, nc, st):
        self.nc = nc
        self.st = st
        self.eng = {}
        for name in ["pe", "act", "dve", "pool", "sp"]:
            sem = st.enter_context(nc.semaphore("s_" + name))
            self.eng[name] = dict(sem=sem, n=0, seen={}, prog=[])
        self.dma = {}
        self.spacer = {}

    def dma_sem(self, name):
        if name not in self.dma:
            self.dma[name] = [self.st.enter_context(self.nc.semaphore("d_" + name)), 0]
        return self.dma[name]

    def op(self, eng, fn, reads=(), writes=(), dma=None):
        E = self.eng[eng]
        deps = {}

        def need(tok):
            if tok is None:
                return
            key, sem, val = tok
            if key == eng and (eng == "pe" or (eng != "pool" and not SELF_SYNC)):
                return
            if key not in deps or deps[key][1] < val:
                deps[key] = (sem, val)

        for b in reads:
            need(b.w)
        for b in writes:
            need(b.w)
            for t in b.r.values():
                need(t)
        if dma is None and eng in self.spacer and not SELF_SYNC:
            last = 0
            for b in list(reads) + list(writes):
                for t in ([b.w] if b.w else []) + list(b.r.values()):
                    if t[0] == eng:
                        last = max(last, t[2])
            if last > 0:
                nsp = GAP - (E["n"] + 1 - last)
                for _ in range(max(0, nsp)):
                    E["n"] += 1
                    E["prog"].append(([], self.spacer[eng], None, E["n"]))
        waits = []
        for key, (sem, val) in deps.items():
            if E["seen"].get(key, 0) < val:
                E["seen"][key] = val
                waits.append((key, sem, val))
        if dma is None:
            E["n"] += 1
            tok = (eng, E["sem"], E["n"])
            E["prog"].append((waits, fn, None, E["n"]))
        else:
            dma = dma + "_" + eng
            d = self.dma_sem(dma)
            d[1] += 16
            tok = ("dma:" + dma, d[0], d[1])
            E["prog"].append((waits, fn, d[0], 0))
        for b in reads:
            b.r[tok[0]] = tok
        for b in writes:
            b.w = tok
            b.r = {}
        return tok

    def wait_tok(self, eng, tok):
        E = self.eng[eng]
        key, sem, val = tok
        if key != eng and E["seen"].get(key, 0) < val:
            E["seen"][key] = val
            E["prog"].append(([(key, sem, val)], None, None, 0))

    def barrier(self):
        toks = [(n, E["sem"], E["n"]) for n, E in self.eng.items() if E["n"] > 0]
        toks += [("dma:" + n, d[0], d[1]) for n, d in self.dma.items() if d[1] > 0]
        for n in self.eng:
            for t in toks:
                self.wait_tok(n, t)

    def emit(self):
        nc = self.nc
        needed = {n: set() for n in self.eng}
        for n, E in self.eng.items():
            for waits, fn, dsem, idx in E["prog"]:
                for (key, sem, val) in waits:
                    if key in needed:
                        needed[key].add(val)
        rank = {n: {v: i + 1 for i, v in enumerate(sorted(vs))} for n, vs in needed.items()}
        self.stats = {n: (E["n"], len(needed[n])) for n, E in self.eng.items()}
        with nc.Block() as block:
            def replay(name):
                def body(e):
                    for waits, fn, dsem, idx in self.eng[name]["prog"]:
                        for (key, sem, val) in waits:
                            e.wait_ge(sem, rank[key][val] if key in rank else val)
                        if fn is not None:
                            ins = fn(e)
                            if dsem is not None:
                                ins.then_inc(dsem, 16)
                            elif idx in needed[name]:
                                ins.then_inc(self.eng[name]["sem"], 1)
                return body
            block.tensor(replay("pe"))
            block.scalar(replay("act"))
            block.vector(replay("dve"))
            block.gpsimd(replay("pool"))
            block.sync(replay("sp"))


def build(mode="fused", nblk=NB1, n_tt=4, stop=99):
    nc = bass.Bass("TRN2", target_bir_lowering=False)
    dr = lambda name, shape, dt=F32, kind="ExternalInput": nc.dram_tensor(name, shape, dt, kind=kind).ap()
    xb = dr("xb", [S, D])
    w1p = dr("w1p", [128, 16, 2176])
    w2a2 = dr("w2a2", [64, 512])
    bias01 = dr("bias01", [1, 512])
    pvec = dr("pvec", [1, 8 * 256])
    pcol = dr("pcol", [128, 48])
    ident_d = dr("ident", [128, 128])
    tri3_d = dr("tri3", [128, 384])
    mask4_d = dr("mask4", [128, 512])
    maskts4_d = dr("maskts4", [128, 512])
    ch_d = dr("ch", [128, 64])
    biasT_d = dr("biasT", [128, 4 * 640])
    maskA_d = dr("maskA", [128, 640])
    if mode == "p1":
        yT_d = dr("yT", [512, S], BF16, kind="ExternalOutput")

    with ExitStack() as st:
        cx = Ctx(nc, st)
        op = cx.op

        def sb(name, shape, dt=F32):
            t = st.enter_context(nc.sbuf_tensor(name, shape, dt))
            return t, Buf(name)

        def ps(name, shape, dt=F32):
            t = st.enter_context(nc.psum_tensor(name, shape, dt))
            return t, Buf(name)

        SCRd, _ = sb("SCRd", [128, 256])
        SCRa, _ = sb("SCRa", [128, 256])
        cx.spacer["dve"] = lambda e: e.memset(SCRd[:, 0:64], 0.0)
        cx.spacer["act"] = lambda e: e.activation(out=SCRa[:, 0:64], in_=SCRd[:, 0:64], func=AF.Copy)
        op("dve", lambda e: e.memset(SCRd[:], 0.0))
        WA, bWA = sb("WA", [128, 16, 2176], BF16)
        WB, bWB = sb("WB", [128, 16, 896], BF16)
        W2, bW2 = sb("W2", [64, 512])
        B01, bB01 = sb("B01", [128, 512])
        ONES1, bONES1 = sb("ONES1", [1, 128])
        PV_, bPV = sb("PV", [128, 5 * 256])
        PC, bPC = sb("PC", [128, 48])
        GS, bGS = sb("GS", [128, 64])
        IDF, bIDF = sb("IDF", [128, 128])
        IDB, bIDB = sb("IDB", [128, 128], BF16)
        TRI, bTRI = sb("TRI", [128, 384])
        MK4, bMK4 = sb("MK4", [128, 512])
        MKTS, bMKTS = sb("MKTS", [128, 512])
        CHt, bCH = sb("CH", [128, 64])
        BM, bBM = sb("BM", [128, 4 * 640], BF16)
        EPS, bEPS = sb("EPS", [128, 2])
        st_outer = st
        st = ExitStack()
        st.__enter__()
        wst = [sb("wst%d" % i, [128, 2176]) for i in range(2)]
        bmst, bbmst = sb("bmst", [128, 4 * 640])
        mast, bmast = sb("mast", [128, 640])
        MU3, bMU3 = sb("MU3", [128, 768])
        OM3, bOM3 = sb("OM3", [128, 768])
        PC8, bPC8 = sb("PC8", [128, 16])

        def ld(eng, dst, bdst, src, name):
            op(eng, lambda e: e.dma_start(out=dst, in_=src), writes=[bdst], dma=name)

        ld("sp", W2[:], bW2, w2a2[:, :], "c0")
        ld("sp", B01[:], bB01, bias01[0:1, :].partition_broadcast(128), "c1")
        ld("sp", PV_[:], bPV, pvec[0:1, 768:2048].partition_broadcast(128), "c2")
        ld("sp", MU3[:], bMU3, pvec[0:1, 0:768].partition_broadcast(128), "c11")
        ld("sp", PC[:], bPC, pcol[:, :], "c3")
        ld("sp", IDF[:], bIDF, ident_d[:, :], "c4")
        ld("sp", TRI[:], bTRI, tri3_d[:, :], "c5")
        ld("sp", MK4[:], bMK4, mask4_d[:, :], "c6")
        ld("sp", MKTS[:], bMKTS, maskts4_d[:, :], "c7")
        ld("sp", CHt[:], bCH, ch_d[:, :], "c8")
        ld("sp", bmst[:], bbmst, biasT_d[:, :], "c9")
        ld("sp", mast[:], bmast, maskA_d[:, :], "c10")

        op("dve", lambda e: e.memset(ONES1[:], 1.0), writes=[bONES1])
        op("dve", lambda e: e.memset(EPS[:, 0:1], 1e-6), writes=[bEPS])
        op("dve", lambda e: e.memset(EPS[:, 1:2], 64e-5), writes=[bEPS])
        op("dve", lambda e: e.tensor_copy(out=IDB[:], in_=IDF[:]), reads=[bIDF], writes=[bIDB])
        op("dve", lambda e: e.tensor_scalar(out=OM3[:], in0=MU3[:], scalar1=-1.0, scalar2=1.0, op0=ALU.mult, op1=ALU.add),
           reads=[bMU3], writes=[bOM3])
        op("dve", lambda e: e.tensor_scalar(out=PC8[:], in0=PC[:, 0:16], scalar1=0.125, scalar2=None, op0=ALU.mult), reads=[bPC], writes=[bPC8])
        for i, (src_c, inv) in enumerate([(16, True), (16, False), (32, True), (32, False)]):
            if inv:
                op("dve", lambda e, i=i, src_c=src_c: e.tensor_scalar(out=GS[:, i * 16:(i + 1) * 16], in0=PC[:, src_c:src_c + 16],
                                                                    scalar1=-1.0, scalar2=1.0, op0=ALU.mult, op1=ALU.add),
                   reads=[bPC], writes=[bGS])
                op("dve", lambda e, i=i: e.tensor_tensor(out=GS[:, i * 16:(i + 1) * 16], in0=GS[:, i * 16:(i + 1) * 16], in1=PC[:, 0:16], op=ALU.mult),
                   reads=[bPC], writes=[bGS])
            else:
                op("dve", lambda e, i=i, src_c=src_c: e.tensor_tensor(out=GS[:, i * 16:(i + 1) * 16], in0=PC[:, src_c:src_c + 16], in1=PC[:, 0:16], op=ALU.mult),
                   reads=[bPC], writes=[bGS])
        op("dve", lambda e: e.tensor_tensor(out=BM[:].rearrange("p (h c) -> p h c", h=4), in0=bmst[:].rearrange("p (h c) -> p h c", h=4),
                                            in1=mast[:].unsqueeze(1).to_broadcast([128, 4, 640]), op=ALU.add),
           reads=[bbmst, bmast], writes=[bBM])
        for kt in range(16):
            wt, bwt = wst[kt % 2]
            ld("sp" if kt % 2 == 0 else "pool", wt[:], bwt, w1p[:, kt, :], "w%d" % (kt % 2))
            g = PC[:, kt:kt + 1]
            op("dve", lambda e, wt=wt, g=g, kt=kt: e.scalar_tensor_tensor(out=WA[:, kt, 0:768], in0=wt[:, 0:768], scalar=g, in1=OM3[:], op0=ALU.mult, op1=ALU.mult),
               reads=[bwt, bOM3, bPC], writes=[bWA])
            op("dve", lambda e, wt=wt, g=g, kt=kt: e.scalar_tensor_tensor(out=WB[:, kt, 0:768], in0=wt[:, 0:768], scalar=g, in1=MU3[:], op0=ALU.mult, op1=ALU.mult),
               reads=[bwt, bMU3, bPC], writes=[bWB])
            for i, (dst, c0) in enumerate([(WA, 768), (WB, 768), (WA, 832), (WB, 832)]):
                op("dve", lambda e, wt=wt, dst=dst, c0=c0, i=i, kt=kt: e.tensor_scalar(out=dst[:, kt, c0:c0 + 64], in0=wt[:, c0:c0 + 64],
                                                                                      scalar1=GS[:, i * 16 + kt:i * 16 + kt + 1], scalar2=None, op0=ALU.mult),
                   reads=[bwt, bGS], writes=[bWA if dst is WA else bWB])
            op("act", lambda e, wt=wt, g=g, kt=kt: e.activation(out=WA[:, kt, 896:1664], in_=wt[:, 896:1664], func=AF.Copy, scale=g),
               reads=[bwt, bPC], writes=[bWA])
            op("dve", lambda e, wt=wt, g=g, kt=kt: e.tensor_scalar(out=WA[:, kt, 1664:1920], in0=wt[:, 1664:1920], scalar1=PC8[:, kt:kt + 1], scalar2=None, op0=ALU.mult),
               reads=[bwt, bPC8], writes=[bWA])
            op("act", lambda e, wt=wt, g=g, kt=kt: e.activation(out=WA[:, kt, 1920:2176], in_=wt[:, 1920:2176], func=AF.Copy, scale=g),
               reads=[bwt, bPC], writes=[bWA])

        cx.barrier()
        st.__exit__(None, None, None)
        st = st_outer
        xt = [sb("xt%d" % i, [128, D]) for i in range(1)]
        xs, bxs = sb("xs", [128, D], BF16)
        st8, bst8 = sb("st8", [128, 8])
        hT = [sb("hT%d" % i, [128, 16, 128], BF16) for i in range(2)]
        hBt, bhB = sb("hB", [128, 16, 128], BF16)
        rk_t, brk = sb("rk_t", [128, 512])
        v_t, bv = sb("v_t", [128, 256])
        vbf, bvbf = sb("vbf", [128, 256], BF16)
        l1T, bl1T = sb("l1T", [64, 256])
        sgin, bsgin = sb("sgin", [128, 512])
        sga, bsga = sb("sga", [128, 512])
        Eg, bEg = sb("Eg", [128, 768])
        gi, bgi = sb("gi", [128, 256])
        gLT, bgLT = sb("gLT", [64, 8])
        szr, bszr = sb("szr", [128, 256])
        sza, bsza = sb("sza", [128, 256])
        qkb, bqkb = sb("qkb", [128, 512], BF16)
        vring, bvring = sb("vring", [128, NR, 256], BF16)
        ONEC, bONEC = sb("ONEC", [128, 2], BF16)
        bvr = [Buf("vr%d" % i) for i in range(NR)]
        kTr, bkTr = sb("kTr", [128, 2, NR, 128], BF16)
        bkr = [Buf("kr%d" % i) for i in range(NR)]
        qT, bqT = sb("qT", [128, 2, 128], BF16)
        tmpA, btmpA = sb("tmpA", [128, 256])
        tmpB, btmpB = sb("tmpB", [128, 256])
        kk_t, bkk = sb("kk_t", [128, 256])
        k_t, bk = sb("k_t", [128, 256])
        ka_t, bka = sb("ka_t", [128, 256])
        s4, bs4 = sb("s4", [128, 16])
        TA, bTA = sb("TA", [128, 1024], BF16)
        btl, bbtl = sb("btl", [128, 256], BF16)
        ktl, bktl = sb("ktl", [128, 256], BF16)
        FT, bFT = sb("FT", [128, 1024], BF16)
        AT = [sb("AT%d" % h, [128, 512], BF16) for h in range(4)]
        XS = [sb("XS%d" % i, [128, 512], BF16) for i in range(2)]
        YS = [sb("YS%d" % i, [128, 512], BF16) for i in range(2)]
        Z = [sb("Z%d" % i, [128, 4, 128], BF16) for i in range(2)]
        DG, bDG = sb("DG", [64, 4, 128])
        PQs, bPQs = sb("PQs", [64, 2, 4, 128])
        RP = [sb("RP%d" % i, [64, 4, 128], BF16) for i in range(2)]
        TS = [sb("TS%d" % i, [64, 4, 64]) for i in range(2)]
        TSb = [sb("TSb%d" % i, [64, 4, 64], BF16) for i in range(2)]
        Yt, bYt = sb("Yt", [128, 256])
        Ysq, bYsq = sb("Ysq", [128, 256])
        yrb, byrb = sb("yrb", [128, 256], BF16)
        PT = [sb("PT%d" % h, [128, 5, 128], BF16) for h in range(2)]
        ya_t, bya = sb("ya_t", [128, 256])
        yab, byab = sb("yab", [128, 256], BF16)
        yTs = [sb("yTs%d" % i, [128, 4, 128], BF16) for i in range(2)]

        PB = [ps("PB%d" % i, [128, 512]) for i in range(3)]
        TBt, bTB = ps("TB", [128, 1024], BF16)
        G = [ps("G%d" % i, [128, 512]) for i in range(4)]

        op("dve", lambda e: e.memset(TS[0][0][:], 0.0), writes=[TS[0][1]])
        op("dve", lambda e: e.memset(TSb[0][0][:], 0.0), writes=[TSb[0][1]])
        op("dve", lambda e: e.memset(ONEC[:], 1.0), writes=[bONEC])
        op("dve", lambda e: e.memset(DG[:], 0.0), writes=[bDG])
        for i in range(2):
            op("dve", lambda e, i=i: e.memset(RP[i][0][:], 0.0), writes=[RP[i][1]])

        PVc = lambda i: PV_[:, i * 256:(i + 1) * 256]
        KKp, KAp, RKp, LNG, LNB = PVc(0), PVc(1), PVc(2), PVc(3), PVc(4)
        h4 = lambda ap: ap.rearrange("p (h n) -> p h n", h=4)
        bc4 = lambda ap: ap.unsqueeze(2).to_broadcast([128, 4, 64])
        gcnt = [0]

        def gbank():
            gcnt[0] += 1
            return G[gcnt[0] % 4]

        def do_block(blk):
            xtile, bx = xt[0]
            hcur, bh = hT[blk % 2]
            hprev, bhp = hT[(blk + 1) % 2]
            ld("sp", xtile[:], bx, xb[blk * 128:(blk + 1) * 128, :], "x%d" % (blk % 2))
            op("dve", lambda e: e.memset(st8[:, 0:1], 0.0), writes=[bst8])
            op("act", lambda e: e.activation(out=xs[:], in_=xtile[:], func=AF.Square, accum_out=st8[:, 0:1]), reads=[bx], writes=[bxs, bst8])
            op("act", lambda e: e.activation(out=st8[:, 1:2], in_=st8[:, 0:1], func=AF.Sqrt, bias=EPS[:, 0:1], scale=1.0 / D), reads=[bEPS], writes=[bst8])
            op("dve", lambda e: e.reciprocal(out=st8[:, 2:3], in_=st8[:, 1:2]), writes=[bst8])
            op("dve", lambda e: e.tensor_scalar(out=xs[:], in0=xtile[:], scalar1=st8[:, 2:3], scalar2=None, op0=ALU.mult), reads=[bx, bst8], writes=[bxs])
            for half in range(2):
                for j in range(8):
                    kt = half * 8 + j
                    op("pe", lambda e, j=j, kt=kt: e.transpose(TBt[:, j * 128:(j + 1) * 128], xs[:, kt * 128:(kt + 1) * 128], IDB[:]),
                       reads=[bxs, bIDB], writes=[bTB])
                op("act" if half == 0 else "dve",
                   (lambda e, half=half: e.activation(out=hcur[:, half * 8:(half + 1) * 8, :], in_=TBt[:].rearrange("p (k t) -> p k t", k=8), func=AF.Copy)) if half == 0 else
                   (lambda e, half=half: e.tensor_copy(out=hcur[:, half * 8:(half + 1) * 8, :], in_=TBt[:].rearrange("p (k t) -> p k t", k=8))),
                   reads=[bTB], writes=[bh])
            if blk == 0:
                op("pool", lambda e: e.memset(hBt[:, :, 0:1], 0.0), writes=[bhB])
            else:
                op("pool", lambda e: e.tensor_copy(out=hBt[:, :, 0:1], in_=hprev[:, :, 127:128]), reads=[bhp], writes=[bhB])
            op("pool", lambda e: e.tensor_copy(out=hBt[:, :, 1:128], in_=hcur[:, :, 0:127]), reads=[bh], writes=[bhB])
            if stop <= 1:
                return
            (P0, bP0), (P1, bP1), (P2, bP2) = PB
            for kt in range(16):
                first = kt == 0
                last = kt == 15
                op("pe", lambda e, kt=kt, first=first: e.matmul(P0[:, :], lhsT=hcur[:, kt, :], rhs=WA[:, kt, 0:512], start=first, stop=False), reads=[bh, bWA], writes=[bP0])
                op("pe", lambda e, kt=kt, first=first: e.matmul(P1[:, 0:256], lhsT=hcur[:, kt, :], rhs=WA[:, kt, 512:768], start=first, stop=False), reads=[bh, bWA], writes=[bP1])
                op("pe", lambda e, kt=kt, last=last: e.matmul(P0[:, :], lhsT=hBt[:, kt, :], rhs=WB[:, kt, 0:512], start=False, stop=last), reads=[bhB, bWB], writes=[bP0])
                op("pe", lambda e, kt=kt, last=last: e.matmul(P1[:, 0:256], lhsT=hBt[:, kt, :], rhs=WB[:, kt, 512:768], start=False, stop=last), reads=[bhB, bWB], writes=[bP1])
            for kt in range(16):
                op("pe", lambda e, kt=kt: e.matmul(P2[:, :], lhsT=hcur[:, kt, :], rhs=WA[:, kt, 896:1408], start=(kt == 0), stop=(kt == 15)), reads=[bh, bWA], writes=[bP2])
            if stop <= 1.2:
                return
            op("dve", lambda e: e.tensor_copy(out=rk_t[:], in_=P0[:, :]), reads=[bP0], writes=[brk])
            op("act", lambda e: e.activation(out=v_t[:], in_=P1[:, 0:256], func=AF.Copy), reads=[bP1], writes=[bv])
            op("dve", lambda e: e.tensor_copy(out=vbf[:], in_=v_t[:]), reads=[bv], writes=[bvbf])
            if stop <= 1.4:
                return
            for kt in range(16):
                op("pe", lambda e, kt=kt: e.matmul(P0[:, :], lhsT=hcur[:, kt, :], rhs=WA[:, kt, 1408:1920], start=(kt == 0), stop=(kt == 15)), reads=[bh, bWA], writes=[bP0])
                op("pe", lambda e, kt=kt: e.matmul(P1[:, 0:256], lhsT=hcur[:, kt, :], rhs=WA[:, kt, 1920:2176], start=(kt == 0), stop=(kt == 15)), reads=[bh, bWA], writes=[bP1])
            if stop <= 1.6:
                return
            slot = blk % NR
            op("act", lambda e: e.activation(out=szr[:], in_=P2[:, 0:256], func=AF.Silu), reads=[bP2], writes=[bszr])
            if stop <= 1.7:
                return
            op("act", lambda e, slot=slot: e.activation(out=vring[:, slot, :], in_=P2[:, 256:512], func=AF.Copy), reads=[bP2], writes=[bvr[slot]])
            if stop <= 1.8:
                return
            op("act", lambda e: e.activation(out=sza[:], in_=P0[:, 0:256], func=AF.Silu), reads=[bP0], writes=[bsza])
            op("act", lambda e: e.activation(out=qkb[:, 0:256], in_=P0[:, 256:512], func=AF.Copy), reads=[bP0], writes=[bqkb])
            op("act", lambda e: e.activation(out=qkb[:, 256:512], in_=P1[:, 0:256], func=AF.Copy), reads=[bP1], writes=[bqkb])

            if stop <= 2:
                return
            (Ga, bGa) = gbank()
            for li in range(2):
                c0 = 768 + li * 64
                for kt in range(16):
                    op("pe", lambda e, Ga=Ga, li=li, c0=c0, kt=kt: e.matmul(Ga[0:64, li * 128:(li + 1) * 128], lhsT=WA[:, kt, c0:c0 + 64], rhs=hcur[:, kt, :], start=(kt == 0), stop=False),
                       reads=[bWA, bh], writes=[bGa])
                    op("pe", lambda e, Ga=Ga, li=li, c0=c0, kt=kt: e.matmul(Ga[0:64, li * 128:(li + 1) * 128], lhsT=WB[:, kt, c0:c0 + 64], rhs=hBt[:, kt, :], start=False, stop=(kt == 15)),
                       reads=[bWB, bhB], writes=[bGa])
            op("act", lambda e, Ga=Ga: e.activation(out=l1T[:, 0:128], in_=Ga[0:64, 0:128], func=AF.Tanh), reads=[bGa], writes=[bl1T])
            op("act", lambda e, Ga=Ga: e.activation(out=l1T[:, 128:256], in_=Ga[0:64, 128:256], func=AF.Copy), reads=[bGa], writes=[bl1T])
            (Gb, bGb) = gbank()
            op("pe", lambda e, Gb=Gb: e.matmul(Gb[:, 0:256], lhsT=l1T[:, 0:128], rhs=W2[:, 0:256], start=True, stop=True), reads=[bl1T, bW2], writes=[bGb])
            op("pe", lambda e, Gb=Gb: e.matmul(Gb[:, 256:512], lhsT=l1T[:, 128:256], rhs=W2[:, 256:512], start=True, stop=True), reads=[bl1T, bW2], writes=[bGb])
            op("dve", lambda e, Gb=Gb: e.tensor_tensor(out=sgin[:], in0=Gb[:, :], in1=B01[:], op=ALU.add), reads=[bGb, bB01], writes=[bsgin])
            op("act", lambda e: e.activation(out=sga[:], in_=sgin[:], func=AF.Sigmoid), reads=[bsgin], writes=[bsga])
            (Gc, bGc) = gbank()
            (Gd, bGd) = gbank()
            op("pe", lambda e, Gc=Gc: e.matmul(Gc[:, 0:256], lhsT=TRI[:, 0:128], rhs=sga[:, 0:256], start=True, stop=True), reads=[bTRI, bsga], writes=[bGc])
            op("pe", lambda e, Gc=Gc: e.matmul(Gc[:, 256:512], lhsT=TRI[:, 128:256], rhs=sga[:, 0:256], start=True, stop=True), reads=[bTRI, bsga], writes=[bGc])
            op("pe", lambda e, Gd=Gd: e.matmul(Gd[:, 0:256], lhsT=TRI[:, 256:384], rhs=sga[:, 0:256], start=True, stop=True), reads=[bTRI, bsga], writes=[bGd])
            for h in range(4):
                op("pe", lambda e, Gd=Gd, h=h: e.matmul(Gd[0:64, 256 + 64 * h:320 + 64 * h], lhsT=sga[:, h * 64:(h + 1) * 64], rhs=CHt[:, :], start=True, stop=True),
                   reads=[bsga, bCH], writes=[bGd])
            op("act", lambda e, Gc=Gc: e.activation(out=Eg[:, 0:512], in_=Gc[:, :], func=AF.Exp, scale=-CDEC), reads=[bGc], writes=[bEg])
            op("act", lambda e, Gc=Gc: e.activation(out=gi[:], in_=Gc[:, 0:256], func=AF.Exp, scale=CDEC), reads=[bGc], writes=[bgi])
            op("act", lambda e, Gd=Gd: e.activation(out=Eg[:, 512:768], in_=Gd[:, 0:256], func=AF.Exp, scale=-CDEC), reads=[bGd], writes=[bEg])
            op("act", lambda e, Gd=Gd: e.activation(out=gLT[:].rearrange("p (h c) -> p h c", h=4), in_=Gd[0:64, 256:512].rearrange("p (h c) -> p h c", h=4)[:, :, 0:2], func=AF.Exp, scale=-CDEC), reads=[bGd], writes=[bgLT])
            if stop <= 3:
                return
            r_ = rk_t[:, 0:256]
            kr_ = rk_t[:, 256:512]
            a_ = sga[:, 256:512]
            gam, gpv, glg = Eg[:, 0:256], Eg[:, 256:512], Eg[:, 512:768]
            dv = lambda fn, reads, writes: op("dve", fn, reads=reads, writes=writes)
            dv(lambda e: e.tensor_tensor(out=tmpA[:], in0=kr_, in1=KKp, op=ALU.mult), [brk, bPV], [btmpA])
            dv(lambda e: e.tensor_tensor(out=tmpB[:], in0=tmpA[:], in1=tmpA[:], op=ALU.mult), [btmpA], [btmpB])
            dv(lambda e: e.tensor_reduce(out=s4[:, 0:4], in_=h4(tmpB[:]), axis=AX.X, op=ALU.add), [btmpB], [bs4])
            op("act", lambda e: e.activation(out=s4[:, 4:8], in_=s4[:, 0:4], func=AF.Sqrt, bias=EPS[:, 0:1], scale=1.0), reads=[bs4, bEPS], writes=[bs4])
            dv(lambda e: e.reciprocal(out=s4[:, 8:12], in_=s4[:, 4:8]), [bs4], [bs4])
            dv(lambda e: e.tensor_tensor(out=h4(kk_t[:]), in0=h4(tmpA[:]), in1=bc4(s4[:, 8:12]), op=ALU.mult), [btmpA, bs4], [bkk])
            dv(lambda e: e.scalar_tensor_tensor(out=tmpB[:], in0=a_, scalar=-1.0, in1=KAp, op0=ALU.add, op1=ALU.mult), [bsga, bPV], [btmpB])
            dv(lambda e: e.scalar_tensor_tensor(out=k_t[:], in0=tmpB[:], scalar=1.0, in1=kr_, op0=ALU.add, op1=ALU.mult), [btmpB, brk], [bk])
            dv(lambda e: e.tensor_tensor(out=ka_t[:], in0=kk_t[:], in1=a_, op=ALU.mult), [bkk, bsga], [bka])
            dv(lambda e: e.scalar_tensor_tensor(out=TA[:, 0:256], in0=kk_t[:], scalar=-1.0, in1=gpv, op0=ALU.mult, op1=ALU.mult), [bkk, bEg], [bTA])
            dv(lambda e: e.tensor_tensor(out=TA[:, 256:512], in0=r_, in1=gam, op=ALU.mult), [brk, bEg], [bTA])
            dv(lambda e: e.tensor_tensor(out=TA[:, 512:768], in0=ka_t[:], in1=gi[:], op=ALU.mult), [bka, bgi], [bTA])
            dv(lambda e: e.tensor_tensor(out=TA[:, 768:1024], in0=k_t[:], in1=gi[:], op=ALU.mult), [bk, bgi], [bTA])
            dv(lambda e: e.tensor_tensor(out=btl[:], in0=ka_t[:], in1=glg, op=ALU.mult), [bka, bEg], [bbtl])
            dv(lambda e: e.tensor_tensor(out=ktl[:], in0=k_t[:], in1=glg, op=ALU.mult), [bk, bEg], [bktl])
            dv(lambda e: e.tensor_tensor(out=tmpA[:], in0=r_, in1=k_t[:], op=ALU.mult), [brk, bk], [btmpA])
            dv(lambda e: e.tensor_tensor(out=tmpA[:], in0=tmpA[:], in1=RKp, op=ALU.mult), [bPV], [btmpA])
            dv(lambda e: e.tensor_reduce(out=s4[:, 12:16], in_=h4(tmpA[:]), axis=AX.X, op=ALU.add), [btmpA], [bs4])
            for p in range(2):
                for qn in range(4):
                    op("pe", lambda e, p=p, qn=qn: e.transpose(TBt[:, p * 512 + qn * 128:p * 512 + (qn + 1) * 128],
                                                              TA[:, qn * 256 + p * 128:qn * 256 + (p + 1) * 128], IDB[:]),
                       reads=[bTA, bIDB], writes=[bTB])
            op("act", lambda e: e.activation(out=FT[:, 0:512], in_=TBt[:, 0:512], func=AF.Copy), reads=[bTB], writes=[bFT])
            op("act", lambda e: e.activation(out=FT[:, 512:1024], in_=TBt[:, 512:1024], func=AF.Copy), reads=[bTB], writes=[bFT])
            if stop <= 4:
                return
            for h in range(4):
                p, j = h // 2, h % 2
                rows = slice(64 * j, 64 * j + 64)
                base = p * 512
                (Gx, bGx) = G[2 + j]
                (Gy, bGy) = G[j]
                op("pe", lambda e, Gx=Gx, rows=rows, base=base: e.matmul(Gx[:, 0:256], lhsT=FT[rows, base + 256:base + 384], rhs=FT[rows, base:base + 256], start=True, stop=True),
                   reads=[bFT], writes=[bGx])
                op("pe", lambda e, Gx=Gx, rows=rows, base=base: e.matmul(Gx[:, 256:512], lhsT=FT[rows, base + 384:base + 512], rhs=FT[rows, base:base + 256], start=True, stop=True),
                   reads=[bFT], writes=[bGx])
                op("pe", lambda e, Gy=Gy, rows=rows, base=base, p=p: e.matmul(Gy[:, p * 128:(p + 1) * 128], lhsT=FT[rows, base:base + 128], rhs=FT[rows, base + 256:base + 384], start=True, stop=True),
                   reads=[bFT], writes=[bGy])
                dv(lambda e, Gx=Gx, h=h: e.tensor_tensor(out=AT[h][0][:], in0=Gx[:, :], in1=MK4[:], op=ALU.mult), [bGx, bMK4], [AT[h][1]])
            for j in range(2):
                (Gy, bGy) = G[j]
                dv(lambda e, Gy=Gy, j=j: e.tensor_tensor(out=YS[0][0][:].rearrange("p (a b t) -> p a b t", a=2, b=2)[:, :, j, :],
                                                         in0=Gy[:, 0:256].rearrange("p (a t) -> p a t", a=2),
                                                         in1=MKTS[:, 0:256].rearrange("p (a t) -> p a t", a=2), op=ALU.mult), [bGy, bMKTS], [YS[0][1]])
            for h in range(4):
                op("pool", lambda e, h=h: e.tensor_copy(out=XS[0][0][:, h * 128:(h + 1) * 128], in_=AT[h][0][:, 0:128]), reads=[AT[h][1]], writes=[XS[0][1]])
            if stop <= 5:
                return
            (Gw, bGw) = gbank()
            for h in range(4):
                op("pe", lambda e, Gw=Gw, h=h: e.matmul(Gw[:, h * 64:(h + 1) * 64], lhsT=AT[h][0][:, 256:384], rhs=vbf[:, h * 64:(h + 1) * 64], start=True, stop=True),
                   reads=[AT[h][1], bvbf], writes=[bGw])
            op("act", lambda e, Gw=Gw: e.activation(out=Z[0][0][:, :, 64:128], in_=h4(Gw[:, 0:256]), func=AF.Copy), reads=[bGw], writes=[Z[0][1]])
            op("pool", lambda e: e.tensor_copy(out=Z[0][0][:, :, 0:64], in_=h4(TA[:, 0:256])), reads=[bTA], writes=[Z[0][1]])
            for lv in range(6):
                Xc, bXc = XS[lv % 2]
                Yc, bYc = YS[lv % 2]
                Xn, bXn = XS[(lv + 1) % 2]
                Yn, bYn = YS[(lv + 1) % 2]
                Zc, bZc = Z[lv % 2]
                Zn, bZn = Z[(lv + 1) % 2]
                (Gz, bGz) = gbank()
                for h in range(4):
                    op("pe", lambda e, Gz=Gz, h=h, Xc=Xc, Zc=Zc: e.matmul(Gz[:, h * 128:(h + 1) * 128], lhsT=Xc[:, h * 128:(h + 1) * 128], rhs=Zc[:, h, :], start=True, stop=False),
                       reads=[bXc, bZc], writes=[bGz])
                    op("pe", lambda e, Gz=Gz, h=h, Zc=Zc: e.matmul(Gz[:, h * 128:(h + 1) * 128], lhsT=IDB[:], rhs=Zc[:, h, :], start=False, stop=True),
                       reads=[bIDB, bZc], writes=[bGz])
                op("act", lambda e, Gz=Gz, Zn=Zn: e.activation(out=Zn[:].rearrange("p h n -> p (h n)"), in_=Gz[:, :], func=AF.Copy), reads=[bGz], writes=[bZn])
                if lv < 5:
                    (G1, bG1) = gbank()
                    (G2, bG2) = gbank()
                    for h in range(4):
                        hs = slice(h * 128, (h + 1) * 128)
                        op("pe", lambda e, G1=G1, hs=hs, Xc=Xc, Yc=Yc: e.matmul(G1[:, hs], lhsT=Yc[:, hs], rhs=Xc[:, hs], start=True, stop=True), reads=[bXc, bYc], writes=[bG1])
                        op("pe", lambda e, G2=G2, hs=hs, Xc=Xc, Yc=Yc: e.matmul(G2[:, hs], lhsT=Xc[:, hs], rhs=Yc[:, hs], start=True, stop=True), reads=[bXc, bYc], writes=[bG2])
                    dv(lambda e, G1=G1, Xn=Xn: e.tensor_copy(out=Xn[:], in_=G1[:, :]), [bG1], [bXn])
                    op("act", lambda e, G2=G2, Yn=Yn: e.activation(out=Yn[:], in_=G2[:, :], func=AF.Copy), reads=[bG2], writes=[bYn])
            if stop <= 6:
                return
            Zf, bZf = Z[0]
            for c in range(2):
                rows = slice(64 * c, 64 * c + 64)
                (Gp, bGp) = gbank()
                for h in range(4):
                    hc = slice(h * 64, (h + 1) * 64)
                    op("pe", lambda e, Gp=Gp, h=h, hc=hc, rows=rows: e.matmul(Gp[0:64, h * 128:h * 128 + 64], lhsT=Zf[rows, h, 0:64], rhs=btl[rows, hc], start=True, stop=True),
                       reads=[bZf, bbtl], writes=[bGp])
                    op("pe", lambda e, Gp=Gp, h=h, hc=hc, rows=rows: e.matmul(Gp[0:64, h * 128 + 64:h * 128 + 128], lhsT=btl[rows, hc], rhs=Zf[rows, h, 64:128], start=True, stop=False),
                       reads=[bZf, bbtl], writes=[bGp])
                    op("pe", lambda e, Gp=Gp, h=h, hc=hc, rows=rows: e.matmul(Gp[0:64, h * 128 + 64:h * 128 + 128], lhsT=ktl[rows, hc], rhs=vbf[rows, hc], start=False, stop=True),
                       reads=[bktl, bvbf], writes=[bGp])
                dv(lambda e, c=c: e.tensor_tensor(out=DG[:, :, 0:64], in0=IDF[0:64, 0:64].unsqueeze(1).to_broadcast([64, 4, 64]),
                                                  in1=gLT[:, c:8:2].unsqueeze(2).to_broadcast([64, 4, 64]), op=ALU.mult), [bIDF, bgLT], [bDG])
                dv(lambda e, c=c, Gp=Gp: e.tensor_tensor(out=PQs[:, c, :, :], in0=Gp[0:64, :].rearrange("p (h n) -> p h n", h=4), in1=DG[:], op=ALU.add), [bGp, bDG], [bPQs])
            (Gr, bGr) = gbank()
            for h in range(4):
                op("pe", lambda e, Gr=Gr, h=h: e.matmul(Gr[0:64, h * 128:(h + 1) * 128], lhsT=Zf[:, h, 0:64], rhs=AT[h][0][:, 128:256], start=True, stop=False),
                   reads=[bZf, AT[h][1]], writes=[bGr])
                op("pe", lambda e, Gr=Gr, h=h: e.matmul(Gr[0:64, h * 128:(h + 1) * 128], lhsT=TA[:, 256 + h * 64:256 + (h + 1) * 64], rhs=IDB[:], start=False, stop=True),
                   reads=[bTA, bIDB], writes=[bGr])
            op("act", lambda e, Gr=Gr: e.activation(out=RP[0][0][:, :, 0:64], in_=Gr[0:64, :].rearrange("p (h n) -> p h n", h=4)[:, :, 0:64], func=AF.Copy), reads=[bGr], writes=[RP[0][1]])
            op("act", lambda e, Gr=Gr: e.activation(out=RP[1][0][:, :, 64:128], in_=Gr[0:64, :].rearrange("p (h n) -> p h n", h=4)[:, :, 64:128], func=AF.Copy), reads=[bGr], writes=[RP[1][1]])
            for c in range(2):
                Tc, bTc = TS[c]
                Tn, bTn = TS[(c + 1) % 2]
                (Gs, bGs_) = gbank()
                for h in range(4):
                    op("pe", lambda e, Gs=Gs, h=h, c=c, Tc=Tc: e.matmul(Gs[0:64, h * 64:(h + 1) * 64], lhsT=PQs[:, c, h, 0:64], rhs=Tc[:, h, :], start=True, stop=False),
                       reads=[bPQs, bTc], writes=[bGs_])
                    op("pe", lambda e, Gs=Gs, h=h, c=c: e.matmul(Gs[0:64, h * 64:(h + 1) * 64], lhsT=IDF[0:64, 0:64], rhs=PQs[:, c, h, 64:128], start=False, stop=True),
                       reads=[bPQs, bIDF], writes=[bGs_])
                if c == 0:
                    op("act", lambda e, Gs=Gs: e.activation(out=TS[1][0][:].rearrange("p h n -> p (h n)"), in_=Gs[0:64, 0:256], func=AF.Copy), reads=[bGs_], writes=[TS[1][1]])
                    dv(lambda e: e.tensor_copy(out=TSb[1][0][:], in_=TS[1][0][:]), [TS[1][1]], [TSb[1][1]])
            (Gq, bGq) = gbank()
            if Gq is Gs:
                (Gq, bGq) = gbank()
            for h in range(4):
                hc = slice(h * 64, (h + 1) * 64)
                op("pe", lambda e, Gq=Gq, h=h, hc=hc: e.matmul(Gq[:, hc], lhsT=AT[h][0][:, 128:256], rhs=Zf[:, h, 64:128], start=True, stop=False), reads=[AT[h][1], bZf], writes=[bGq])
                op("pe", lambda e, Gq=Gq, h=h, hc=hc: e.matmul(Gq[:, hc], lhsT=AT[h][0][:, 384:512], rhs=vbf[:, hc], start=False, stop=False), reads=[AT[h][1], bvbf], writes=[bGq])
                op("pe", lambda e, Gq=Gq, h=h, hc=hc: e.matmul(Gq[:, hc], lhsT=RP[0][0][:, h, :], rhs=TSb[0][0][:, h, :], start=False, stop=False), reads=[RP[0][1], TSb[0][1]], writes=[bGq])
                op("pe", lambda e, Gq=Gq, h=h, hc=hc: e.matmul(Gq[:, hc], lhsT=RP[1][0][:, h, :], rhs=TSb[1][0][:, h, :], start=False, stop=True), reads=[RP[1][1], TSb[1][1]], writes=[bGq])
            op("act", lambda e, Gs=Gs: e.activation(out=TS[0][0][:].rearrange("p h n -> p (h n)"), in_=Gs[0:64, 0:256], func=AF.Copy), reads=[bGs_], writes=[TS[0][1]])
            dv(lambda e: e.tensor_copy(out=TSb[0][0][:], in_=TS[0][0][:]), [TS[0][1]], [TSb[0][1]])
            if stop <= 7:
                return
            op("act", lambda e, Gq=Gq: e.activation(out=Yt[:], in_=Gq[:, 0:256], func=AF.Copy), reads=[bGq], writes=[bYt])
            dv(lambda e: e.tensor_reduce(out=s4[:, 0:4], in_=h4(Yt[:]), axis=AX.X, op=ALU.add), [bYt], [bs4])
            dv(lambda e: e.tensor_tensor(out=Ysq[:], in0=Yt[:], in1=Yt[:], op=ALU.mult), [bYt], [bYsq])
            dv(lambda e: e.tensor_reduce(out=s4[:, 4:8], in_=h4(Ysq[:]), axis=AX.X, op=ALU.add), [bYsq], [bs4])
            dv(lambda e: e.tensor_scalar(out=s4[:, 0:4], in0=s4[:, 0:4], scalar1=1.0 / 64, scalar2=None, op0=ALU.mult), [bs4], [bs4])
            dv(lambda e: e.tensor_tensor(out=s4[:, 8:12], in0=s4[:, 0:4], in1=s4[:, 0:4], op=ALU.mult), [bs4], [bs4])
            dv(lambda e: e.scalar_tensor_tensor(out=s4[:, 4:8], in0=s4[:, 4:8], scalar=1.0 / 64, in1=s4[:, 8:12], op0=ALU.mult, op1=ALU.subtract), [bs4], [bs4])
            op("act", lambda e: e.activation(out=s4[:, 8:12], in_=s4[:, 4:8], func=AF.Sqrt, bias=EPS[:, 1:2], scale=1.0), reads=[bs4, bEPS], writes=[bs4])
            dv(lambda e: e.reciprocal(out=s4[:, 4:8], in_=s4[:, 8:12]), [bs4], [bs4])
            dv(lambda e: e.tensor_tensor(out=h4(Yt[:]), in0=h4(Yt[:]), in1=bc4(s4[:, 0:4]), op=ALU.subtract), [bs4], [bYt])
            dv(lambda e: e.tensor_tensor(out=h4(Yt[:]), in0=h4(Yt[:]), in1=bc4(s4[:, 4:8]), op=ALU.mult), [bs4], [bYt])
            dv(lambda e: e.tensor_tensor(out=Yt[:], in0=Yt[:], in1=LNG, op=ALU.mult), [bPV], [bYt])
            dv(lambda e: e.tensor_tensor(out=Yt[:], in0=Yt[:], in1=LNB, op=ALU.add), [bPV], [bYt])
            dv(lambda e: e.tensor_tensor(out=h4(Ysq[:]), in0=h4(v_t[:]), in1=bc4(s4[:, 12:16]), op=ALU.mult), [bv, bs4], [bYsq])
            dv(lambda e: e.tensor_tensor(out=Yt[:], in0=Yt[:], in1=Ysq[:], op=ALU.add), [bYsq], [bYt])
            dv(lambda e: e.tensor_tensor(out=yrb[:], in0=Yt[:], in1=szr[:], op=ALU.mult), [bYt, bszr], [byrb])

            if stop <= 8:
                return
            for p in range(2):
                op("pe", lambda e, p=p: e.transpose(TBt[:, p * 128:(p + 1) * 128], qkb[:, p * 128:(p + 1) * 128], IDB[:]), reads=[bqkb, bIDB], writes=[bTB])
                op("pe", lambda e, p=p: e.transpose(TBt[:, 256 + p * 128:256 + (p + 1) * 128], qkb[:, 256 + p * 128:256 + (p + 1) * 128], IDB[:]), reads=[bqkb, bIDB], writes=[bTB])
            op("act", lambda e: e.activation(out=qT[:].rearrange("p a t -> p (a t)"), in_=TBt[:, 0:256], func=AF.Copy), reads=[bTB], writes=[bqT])
            op("act", lambda e, slot=slot: e.activation(out=kTr[:, :, slot, :], in_=TBt[:, 256:512].rearrange("p (a t) -> p a t", a=2), func=AF.Copy), reads=[bTB], writes=[bkr[slot]])
            m0 = max(0, 4 - blk)
            (Go, bGo) = gbank()
            for h in range(4):
                p, j = h // 2, h % 2
                rows = slice(64 * j, 64 * j + 64)
                PTh, bPTh = PT[h % 2]
                (Gs1, bGs1) = gbank()
                if Gs1 is Go:
                    (Gs1, bGs1) = gbank()
                (Gs2, bGs2) = gbank()
                if Gs2 is Go:
                    (Gs2, bGs2) = gbank()
                for m in range(m0, 5):
                    kb = blk - 4 + m
                    sl = kb % NR
                    Gt, bGt, cc = (Gs1, bGs1, m * 128) if m < 4 else (Gs2, bGs2, 0)
                    op("pe", lambda e, Gt=Gt, cc=cc, rows=rows, p=p, sl=sl: e.matmul(Gt[:, cc:cc + 128], lhsT=kTr[rows, p, sl, :], rhs=qT[rows, p, :], start=True, stop=False),
                       reads=[bkr[sl], bqT], writes=[bGt])
                    op("pe", lambda e, Gt=Gt, cc=cc, h=h, m=m: e.matmul(Gt[:, cc:cc + 128], lhsT=IDB[:], rhs=BM[:, h * 640 + m * 128:h * 640 + (m + 1) * 128], start=False, stop=True),
                       reads=[bIDB, bBM], writes=[bGt])
                if m0 < 4:
                    op("act", lambda e, Gs1=Gs1, PTh=PTh, m0=m0: e.activation(out=PTh[:, m0:4, :], in_=Gs1[:, m0 * 128:512].rearrange("p (m t) -> p m t", t=128), func=AF.Exp),
                       reads=[bGs1], writes=[bPTh])
                op("act", lambda e, Gs2=Gs2, PTh=PTh: e.activation(out=PTh[:, 4, :], in_=Gs2[:, 0:128], func=AF.Exp), reads=[bGs2], writes=[bPTh])
                for m in range(m0, 5):
                    kb = blk - 4 + m
                    sl = kb % NR
                    op("pe", lambda e, Go=Go, h=h, m=m, sl=sl, PTh=PTh: e.matmul(Go[:, h * 65:h * 65 + 64], lhsT=PTh[:, m, :], rhs=vring[:, sl, h * 64:(h + 1) * 64], start=(m == m0), stop=(m == 4)),
                       reads=[bPTh, bvr[sl]], writes=[bGo])

                for m in range(m0, 5):
                    op("pe", lambda e, Go=Go, h=h, m=m, PTh=PTh: e.matmul(Go[:, h * 65 + 64:h * 65 + 65], lhsT=PTh[:, m, :], rhs=ONEC[:, 0:1], start=(m == m0), stop=(m == 4)),
                       reads=[bPTh, bONEC], writes=[bGo])
            Go3 = Go[:, 0:260].rearrange("p (h n) -> p h n", h=4)
            dv(lambda e, Go3=Go3: e.reciprocal(out=s4[:, 0:4], in_=Go3[:, :, 64]), [bGo], [bs4])
            dv(lambda e, Go3=Go3: e.tensor_tensor(out=h4(ya_t[:]), in0=Go3[:, :, 0:64], in1=bc4(s4[:, 0:4]), op=ALU.mult), [bGo, bs4], [bya])
            dv(lambda e: e.tensor_tensor(out=yab[:], in0=ya_t[:], in1=sza[:], op=ALU.mult), [bya, bsza], [byab])
            if stop <= 9:
                return
            yT_s, byT = yTs[blk % 2]
            for p in range(2):
                op("pe", lambda e, p=p: e.transpose(TBt[:, p * 128:(p + 1) * 128], yrb[:, p * 128:(p + 1) * 128], IDB[:]), reads=[byrb, bIDB], writes=[bTB])
                op("pe", lambda e, p=p: e.transpose(TBt[:, 256 + p * 128:256 + (p + 1) * 128], yab[:, p * 128:(p + 1) * 128], IDB[:]), reads=[byab, bIDB], writes=[bTB])
            op("act", lambda e, yT_s=yT_s: e.activation(out=yT_s[:].rearrange("p a t -> p (a t)"), in_=TBt[:, 0:512], func=AF.Copy), reads=[bTB], writes=[byT])
            op("pool", lambda e, yT_s=yT_s, blk=blk: e.dma_start(out=yT_d[:, blk * 128:(blk + 1) * 128].rearrange("(a p) t -> p a t", p=128), in_=yT_s[:]),
               reads=[byT], dma="yo%d" % (blk % 2))

        for blk_ in range(nblk):
            do_block(blk_)
        if os.environ.get("K_DBG") == "1":
            dl = [("xs", xs, bxs, [128, 2048], BF16), ("hA", hT[0][0], hT[0][1], [128, 16, 128], BF16), ("hB", hBt, bhB, [128, 16, 128], BF16),
                  ("rk", rk_t, brk, [128, 512], F32), ("v", v_t, bv, [128, 256], F32), ("l1T", l1T, bl1T, [64, 256], F32), ("sga", sga, bsga, [128, 512], F32),
                  ("Eg", Eg, bEg, [128, 768], F32), ("gi", gi, bgi, [128, 256], F32), ("gLT", gLT, bgLT, [64, 8], F32), ("szr", szr, bszr, [128, 256], F32),
                  ("sza", sza, bsza, [128, 256], F32), ("qkb", qkb, bqkb, [128, 512], BF16), ("TA", TA, bTA, [128, 1024], BF16), ("FT", FT, bFT, [128, 1024], BF16),
                  ("AT0", AT[0][0], AT[0][1], [128, 512], BF16), ("AT1", AT[1][0], AT[1][1], [128, 512], BF16), ("Z0", Z[0][0], Z[0][1], [128, 4, 128], BF16),
                  ("PQs", PQs, bPQs, [64, 2, 4, 128], F32), ("RP0", RP[0][0], RP[0][1], [64, 4, 128], BF16), ("TS0", TS[0][0], TS[0][1], [64, 4, 64], F32),
                  ("Yt", Yt, bYt, [128, 256], F32), ("yrb", yrb, byrb, [128, 256], BF16), ("qT", qT, bqT, [128, 2, 128], BF16),
                  ("ya", ya_t, bya, [128, 256], F32), ("yab", yab, byab, [128, 256], BF16),
                  ("WA0", WA, bWA, [128, 16, 2176], BF16), ("XS0", XS[0][0], XS[0][1], [128, 512], BF16),
                  ("YS0", YS[0][0], YS[0][1], [128, 512], BF16), ("BM", BM, bBM, [128, 2560], BF16)]
            for (nm, t, bb, shp, dt_) in dl:
                dd = dr("dbg_" + nm, shp, dt_, kind="ExternalOutput")
                extra = bkr + bvr if nm in ("kTr", "vring") else []
                op("sp", lambda e, dd=dd, t=t: e.dma_start(out=dd, in_=t[:]), reads=[bb] + extra, dma="yo_dbg")

        for name, (sem, cnt) in cx.dma.items():
            if name.startswith("yo"):
                cx.wait_tok("sp", ("dma:" + name, sem, cnt))
        cx.emit()
    return nc


def _consts():
    idx = np.arange(128)
    same = (idx[:, None] // 64) == (idx[None, :] // 64)
    incl = ((idx[:, None] <= idx[None, :]) & same).astype(np.float32)
    excl = ((idx[:, None] < idx[None, :]) & same).astype(np.float32)
    suf = ((idx[:, None] > idx[None, :]) & same).astype(np.float32)
    c = {}
    c["ident"] = np.eye(128, dtype=np.float32)
    c["tri3"] = np.ascontiguousarray(np.concatenate([incl, excl, suf], 1))
    c["mask4"] = np.ascontiguousarray(np.concatenate([excl, incl, excl, incl], 1))
    c["maskts4"] = np.ascontiguousarray(np.concatenate([suf] * 4, 1))
    c["ch"] = np.zeros((128, 64), np.float32)
    c["ch"][0:64, 0] = 1.0
    c["ch"][64:128, 1] = 1.0
    mA = np.zeros((128, 5, 128), np.float32)
    mA[0:64, 0, 64:128] = NEG
    mA[64:128, 4, 0:64] = NEG
    c["maskA"] = np.ascontiguousarray(mA.reshape(128, 640))
    kk = idx[:, None, None]
    m = np.arange(5)[None, :, None]
    qq = idx[None, None, :]
    rel = 128 * (4 - m) + qq - kk
    c["relidx"] = np.clip(rel, -256, 256) + 256
    return c


def _p1_inputs(inp, core):
    b, g = core // 4, core % 4
    c = _consts()
    w_in = inp["w_in"][0]
    hs = slice(g * 256, (g + 1) * 256)
    col = lambda i: w_in[:, i * 1024 + g * 256: i * 1024 + (g + 1) * 256]
    wcat = np.concatenate([col(0), col(1), col(2), inp["w1"][0], inp["a1"][0], col(3), col(6), col(7), col(4), col(5)], axis=1)
    m = {}
    m["xb"] = np.ascontiguousarray(inp["x"][b])
    m["w1p"] = np.ascontiguousarray(wcat.reshape(16, 128, 2176).transpose(1, 0, 2))
    m["w2a2"] = np.ascontiguousarray(np.concatenate([inp["w2"][0][:, hs], inp["a2"][0][:, hs]], 1))
    m["bias01"] = np.ascontiguousarray(np.concatenate([inp["w0"][0][hs], inp["a0"][0][hs]])[None, :])
    pv = [inp["mu_rkv"][0][0][hs], inp["mu_rkv"][0][1][hs], inp["mu_rkv"][0][2][hs], inp["k_k"][0][hs], inp["k_a"][0][hs],
          inp["r_k"][0].reshape(-1)[hs], inp["ln_x_g"][0][hs], inp["ln_x_b"][0][hs]]
    m["pvec"] = np.ascontiguousarray(np.concatenate(pv)[None, :])
    pc = [inp["pre_norm_g"][0].reshape(16, 128).T, inp["mu_wa"][0][0].reshape(16, 128).T, inp["mu_wa"][0][1].reshape(16, 128).T]
    m["pcol"] = np.ascontiguousarray(np.concatenate(pc, 1))
    for k in ["ident", "tri3", "mask4", "maskts4", "ch", "maskA"]:
        m[k] = c[k]
    rb = inp["rel_bias"][0][g * 4:(g + 1) * 4]
    bt = rb[:, c["relidx"]]
    m["biasT"] = np.ascontiguousarray(bt.transpose(1, 0, 2, 3).reshape(128, 4 * 640))
    return {k: np.asarray(v, dtype=np.float32) for k, v in m.items()}


def kernel(**inputs):
    inp = {k: np.asarray(v) for k, v in inputs.items()}
    nc1 = build("p1")
    in_maps = [_p1_inputs(inp, c) for c in range(8)]
    res1 = run_bass_kernel_spmd(nc1, in_maps, core_ids=list(range(8)))
    yT = [np.asarray(res1.results[c]["yT"]) for c in range(8)]
    nc2 = build_p2()
    in_maps2 = [_p2_inputs(inp, c, yT) for c in range(8)]
    res2 = run_bass_kernel_spmd(nc2, in_maps2, core_ids=list(range(8)))
    out = np.empty((2, S, D), np.float32)
    for c in range(8):
        b_, g_ = c // 4, c % 4
        out[b_, g_ * 2048:(g_ + 1) * 2048] = np.asarray(res2.results[c]["out"])
    return out


TT = 512


def build_p2(n_tt=4, stop=99):
    nc = bass.Bass("TRN2", target_bir_lowering=False)
    dr = lambda name, shape, dt=F32, kind="ExternalInput": nc.dram_tensor(name, shape, dt, kind=kind).ap()
    xq = dr("xq", [2048, D])
    yTall = dr("yTall", [2048, 2048], mybir.dt.uint16).bitcast(BF16)
    wg = dr("wg", [16, 2, 128, 2048])
    wb = dr("wb", [16, 128, 2048])
    wout = dr("wout", [128, 16, 2048])
    bmc = dr("bmc", [128, 32])
    gpost = dr("gpost", [1, 2048])
    pcol = dr("pcol2", [128, 16])
    ident_d = dr("ident", [128, 128])
    out_d = dr("out", [2048, D], kind="ExternalOutput")
    with ExitStack() as st:
        cx = Ctx(nc, st)
        phase2_body(nc, cx, st, xq, yTall, wg, wb, wout, bmc, gpost, pcol, ident_d, out_d, n_tt, stop)
        for name, (sem, cnt) in cx.dma.items():
            if name.startswith("oo"):
                cx.wait_tok("sp", ("dma:" + name, sem, cnt))
        cx.emit()
    return nc


def phase2_body(nc, cx, st, xq, yTall, wg, wb, wout, bmc, gpost, pcol, ident_d, out_d, n_tt, stop=99):
    op = cx.op

    def sb(name, shape, dt=F32):
        t = st.enter_context(nc.sbuf_tensor(name, shape, dt))
        return t, Buf(name)

    def ps(name, shape, dt=F32):
        t = st.enter_context(nc.psum_tensor(name, shape, dt))
        return t, Buf(name)

    def ld(eng, dst, bdst, src, name):
        op(eng, lambda e: e.dma_start(out=dst, in_=src), writes=[bdst], dma=name)

    SCRd, _ = sb("SCRd2", [128, 256])
    SCRa, _ = sb("SCRa2", [128, 256])
    cx.spacer["dve"] = lambda e: e.memset(SCRd[:, 0:64], 0.0)
    cx.spacer["act"] = lambda e: e.activation(out=SCRa[:, 0:64], in_=SCRd[:, 0:64], func=AF.Copy)
    op("dve", lambda e: e.memset(SCRd[:], 0.0))
    WO, bWO = sb("WO", [128, 16, 2048], BF16)
    GP, bGP = sb("GP", [128, 2048])
    BMc, bBMc = sb("BMc", [128, 32])
    PCg, bPCg = sb("PCg", [128, 16])
    IDF, bIDF = sb("IDF2", [128, 128])
    IDB, bIDB = sb("IDB2", [128, 128], BF16)
    EPS, bEPS = sb("EPS2", [128, 1])
    hT2, bhT2 = sb("hT2", [128, 16, TT], BF16)
    yT2, byT2 = sb("yT2", [128, 16, TT], BF16)
    mT, bmT = sb("mT", [128, 16, TT], BF16)
    wst = [sb("wst2_%d" % i, [128, 2048]) for i in range(2)]
    wgb = [sb("wgb%d" % i, [128, 2, 2048], BF16) for i in range(2)]
    wbb = [sb("wbb%d" % i, [128, 2048], BF16) for i in range(2)]
    xt, bxt = sb("xt2", [128, D])
    xs, bxs = sb("xs2", [128, D], BF16)
    ot, bot = sb("ot2", [128, D])
    st8, bst8 = sb("st8_2", [128, 8])
    sr, bsr = sb("sr", [128, TT])
    sa, bsa = sb("sa", [128, TT])
    t1, bt1 = sb("t1", [128, TT])
    t2, bt2 = sb("t2", [128, TT])
    Q = [ps("Q%d" % i, [128, 512]) for i in range(4)]
    QT, bQT = ps("QT", [128, 1024], BF16)

    ld("sp", GP[:], bGP, gpost[0:1, :].partition_broadcast(128), "k0")
    ld("sp", BMc[:], bBMc, bmc[:, :], "k1")
    ld("sp", PCg[:], bPCg, pcol[:, :], "k2")
    ld("sp", IDF[:], bIDF, ident_d[:, :], "k3")
    op("dve", lambda e: e.tensor_copy(out=IDB[:], in_=IDF[:]), reads=[bIDF], writes=[bIDB])
    op("dve", lambda e: e.memset(EPS[:], 1e-6), writes=[bEPS])
    for kt in range(16):
        wt, bwt = wst[kt % 2]
        ld("sp" if kt % 2 == 0 else "pool", wt[:], bwt, wout[:, kt, :], "ws%d" % (kt % 2))
        if kt % 2 == 0:
            op("act", lambda e, wt=wt, kt=kt: e.activation(out=WO[:, kt, :], in_=wt[:], func=AF.Copy), reads=[bwt], writes=[bWO])
        else:
            op("dve", lambda e, wt=wt, kt=kt: e.tensor_copy(out=WO[:, kt, :], in_=wt[:]), reads=[bwt], writes=[bWO])

    def norm_block(r0):
        ld("sp", xt[:], bxt, xq[r0:r0 + 128, :], "x2")
        op("dve", lambda e: e.memset(st8[:, 0:1], 0.0), writes=[bst8])
        op("act", lambda e: e.activation(out=xs[:], in_=xt[:], func=AF.Square, accum_out=st8[:, 0:1]), reads=[bxt], writes=[bxs, bst8])
        op("act", lambda e: e.activation(out=st8[:, 1:2], in_=st8[:, 0:1], func=AF.Sqrt, bias=EPS[:, 0:1], scale=1.0 / D), reads=[bEPS, bst8], writes=[bst8])
        op("dve", lambda e: e.reciprocal(out=st8[:, 2:3], in_=st8[:, 1:2]), reads=[bst8], writes=[bst8])

    def do_tile(tt):
        tok0 = tt * TT
        if stop <= 0:
            return
        for sub in range(4):
            norm_block(tok0 + sub * 128)
            op("dve", lambda e: e.tensor_scalar(out=xs[:], in0=xt[:], scalar1=st8[:, 2:3], scalar2=None, op0=ALU.mult), reads=[bxt, bst8], writes=[bxs])
            for half in range(2):
                for j in range(8):
                    kt = half * 8 + j
                    op("pe", lambda e, j=j, kt=kt: e.transpose(QT[:, j * 128:(j + 1) * 128], xs[:, kt * 128:(kt + 1) * 128], IDB[:]), reads=[bxs, bIDB], writes=[bQT])
                if half == 0:
                    op("act", lambda e, half=half, sub=sub: e.activation(out=hT2[:, half * 8:(half + 1) * 8, sub * 128:(sub + 1) * 128],
                                                                         in_=QT[:].rearrange("p (k t) -> p k t", k=8), func=AF.Copy), reads=[bQT], writes=[bhT2])
                else:
                    op("dve", lambda e, half=half, sub=sub: e.tensor_copy(out=hT2[:, half * 8:(half + 1) * 8, sub * 128:(sub + 1) * 128],
                                                                          in_=QT[:].rearrange("p (k t) -> p k t", k=8)), reads=[bQT], writes=[bhT2])
        if stop <= 1:
            return
        for br in range(2):
            for rk in range(4):
                r0 = rk * 512 + br * 256
                ld("pool", yT2[:, br * 8 + rk * 2:br * 8 + rk * 2 + 2, :], byT2,
                   yTall[r0:r0 + 256, tok0:tok0 + TT].rearrange("(a p) t -> p a t", p=128), "y2")
        if stop <= 2:
            return
        for j in range(16):
            if stop <= 3 and j >= 1:
                break
            wgt, bwg = wgb[j % 2]
            wbt, bwb = wbb[j % 2]
            for ra in range(2):
                wt, bwt = wst[ra]
                ld("sp", wt[:], bwt, wg[j, ra, :, :], "ws%d" % ra)
                op("dve",
                   lambda e, wt=wt, wgt=wgt, ra=ra: e.tensor_tensor(out=wgt[:, ra, :].rearrange("p (k c) -> p k c", k=16), in0=wt[:].rearrange("p (k c) -> p k c", k=16),
                                                                    in1=PCg[:].unsqueeze(2).to_broadcast([128, 16, 128]), op=ALU.mult),
                   reads=[bwt, bPCg], writes=[bwg])
            wt, bwt = wst[0]
            ld("pool", wt[:], bwt, wb[j, :, :], "ws0")
            op("act", lambda e, wt=wt, wbt=wbt: e.activation(out=wbt[:], in_=wt[:], func=AF.Copy), reads=[bwt], writes=[bwb])
            for ra in range(2):
                Qx, bQx = Q[ra]
                for kt in range(16):
                    op("pe", lambda e, Qx=Qx, ra=ra, kt=kt, wgt=wgt: e.matmul(Qx[:, :], lhsT=wgt[:, ra, kt * 128:(kt + 1) * 128], rhs=hT2[:, kt, :], start=(kt == 0), stop=(kt == 15)),
                       reads=[bwg, bhT2], writes=[bQx])
            for br in range(2):
                Qx, bQx = Q[2 + br]
                for kt in range(8):
                    op("pe", lambda e, Qx=Qx, br=br, kt=kt, wbt=wbt: e.matmul(Qx[:, :], lhsT=wbt[:, (br * 8 + kt) * 128:(br * 8 + kt + 1) * 128], rhs=yT2[:, br * 8 + kt, :], start=(kt == 0), stop=(kt == 7)),
                       reads=[bwb, byT2], writes=[bQx])
            op("act", lambda e, j=j: e.activation(out=sr[:], in_=Q[0][0][:, :], func=AF.Sigmoid, bias=BMc[:, j:j + 1], scale=1.0), reads=[Q[0][1], bBMc], writes=[bsr])
            op("act", lambda e, j=j: e.activation(out=sa[:], in_=Q[1][0][:, :], func=AF.Sigmoid, bias=BMc[:, 16 + j:17 + j], scale=1.0), reads=[Q[1][1], bBMc], writes=[bsa])
            op("dve", lambda e: e.tensor_tensor(out=t1[:], in0=sr[:], in1=Q[2][0][:, :], op=ALU.mult), reads=[bsr, Q[2][1]], writes=[bt1])
            op("dve", lambda e: e.tensor_tensor(out=t2[:], in0=sa[:], in1=Q[3][0][:, :], op=ALU.mult), reads=[bsa, Q[3][1]], writes=[bt2])
            op("dve", lambda e, j=j: e.tensor_tensor(out=mT[:, j, :], in0=t1[:], in1=t2[:], op=ALU.add), reads=[bt1, bt2], writes=[bmT])
        if stop <= 4:
            return
        for sub in range(4):
            r0 = tok0 + sub * 128
            for cb in range(4):
                Qx, bQx = Q[cb]
                for kt in range(16):
                    op("pe", lambda e, Qx=Qx, cb=cb, kt=kt, sub=sub: e.matmul(Qx[:, :], lhsT=mT[:, kt, sub * 128:(sub + 1) * 128], rhs=WO[:, kt, cb * 512:(cb + 1) * 512], start=(kt == 0), stop=(kt == 15)),
                       reads=[bmT, bWO], writes=[bQx])
            op("dve", lambda e: e.memset(st8[:, 4:8], 0.0), writes=[bst8])
            for cb in range(4):
                op("act", lambda e, cb=cb: e.activation(out=xs[:, cb * 512:(cb + 1) * 512], in_=Q[cb][0][:, :], func=AF.Square, accum_out=st8[:, 4 + cb:5 + cb]),
                   reads=[Q[cb][1]], writes=[bxs, bst8])
            op("dve", lambda e: e.tensor_reduce(out=st8[:, 0:1], in_=st8[:, 4:8], axis=AX.X, op=ALU.add), reads=[bst8], writes=[bst8])
            op("act", lambda e: e.activation(out=st8[:, 1:2], in_=st8[:, 0:1], func=AF.Sqrt, bias=EPS[:, 0:1], scale=1.0 / D), reads=[bEPS, bst8], writes=[bst8])
            op("dve", lambda e: e.reciprocal(out=st8[:, 2:3], in_=st8[:, 1:2]), reads=[bst8], writes=[bst8])
            ld("sp", xt[:], bxt, xq[r0:r0 + 128, :], "x2")
            for cb in range(4):
                cs = slice(cb * 512, (cb + 1) * 512)
                op("dve", lambda e, cb=cb, cs=cs: e.scalar_tensor_tensor(out=ot[:, cs], in0=Q[cb][0][:, :], scalar=st8[:, 2:3], in1=GP[:, cs], op0=ALU.mult, op1=ALU.mult),
                   reads=[Q[cb][1], bst8, bGP], writes=[bot])
            op("dve", lambda e: e.tensor_tensor(out=ot[:], in0=ot[:], in1=xt[:], op=ALU.add), reads=[bxt], writes=[bot])
            op("sp", lambda e, r0=r0: e.dma_start(out=out_d[r0:r0 + 128, :], in_=ot[:]), reads=[bot], dma="oo")

    for tt in range(n_tt):
        do_tile(tt)


def _p2_inputs(inp, core, yT_by_core):
    b, g = core // 4, core % 4
    w_in = inp["w_in"][0]
    m = {}
    m["xq"] = np.ascontiguousarray(inp["x"][b, g * 2048:(g + 1) * 2048])
    m["yTall"] = np.ascontiguousarray(np.concatenate([yT_by_core[b * 4 + r][:, g * 2048:(g + 1) * 2048] for r in range(4)], 0)).view(np.uint16)
    gates = w_in[:, 8192:12288].reshape(16, 128, 2, 16, 128)
    m["wg"] = np.ascontiguousarray(gates.transpose(3, 2, 1, 0, 4).reshape(16, 2, 128, 2048))
    wbr = inp["w_branch_rwkv"][0].reshape(8, 128, 16, 128)
    wba = inp["w_branch_attn"][0].reshape(8, 128, 16, 128)
    wbcat = np.stack([wbr, wba], 0)
    m["wb"] = np.ascontiguousarray(wbcat.transpose(3, 2, 0, 1, 4).reshape(16, 128, 2048))
    m["wout"] = np.ascontiguousarray(inp["w_out"][0].reshape(16, 128, 2048).transpose(1, 0, 2))
    bm = inp["b_merge"][0].reshape(2, 16, 128)
    m["bmc"] = np.ascontiguousarray(bm.transpose(2, 0, 1).reshape(128, 32))
    m["gpost"] = np.ascontiguousarray(inp["post_norm_g"][0][None, :])
    m["pcol2"] = np.ascontiguousarray(inp["pre_norm_g"][0].reshape(16, 128).T)
    m["ident"] = np.eye(128, dtype=np.float32)
    return m
```
